# Optimizing a Trainium2 kernel written in Bass

```python
import math
import jax, jax.numpy as jnp
from jax import lax
import numpy as np

D_MODEL = 4096
BATCH = 2
SEQ = 8192
DEPTH = 1

ATT_HEADS = 32
ATT_KV_HEADS = 4
ATT_GROUP = ATT_HEADS // ATT_KV_HEADS
ATT_HEAD_DIM = 64
WINDOW = 128
ATT_BLOCK = 128
RET_HEADS = 8
RET_KEY_DIM = 128
RET_VAL_DIM = 256
RET_CHUNK = 128
ATT_Q_WIDTH = ATT_HEADS * ATT_HEAD_DIM
ATT_KV_WIDTH = ATT_KV_HEADS * ATT_HEAD_DIM
RET_QK_WIDTH = RET_HEADS * RET_KEY_DIM
RET_V_WIDTH = RET_HEADS * RET_VAL_DIM
IN_SPLITS = [ATT_Q_WIDTH, ATT_KV_WIDTH, ATT_KV_WIDTH, RET_QK_WIDTH, RET_QK_WIDTH, RET_V_WIDTH, RET_V_WIDTH, D_MODEL, D_MODEL]
IN_OFFSETS = [int(v) for v in np.cumsum(IN_SPLITS)[:-1]]
IN_WIDTH = int(sum(IN_SPLITS))
N_EXPERTS = 32
TOP_K = 4
EXPERT_FF = 1536
EXPERT_BLOCK = 256
SWIGLU_LIMIT = 7.0
SWIGLU_ALPHA = 1.702
PLE_DIM = 256
LN_EPS = 1e-5
DEEPNORM_ALPHA = float((2 * DEPTH) ** 0.25)
DEEPNORM_BETA = float((8 * DEPTH) ** -0.25)

kernel_name = 'hybrid_swa_retention_moe_block'


def _layer_norm(x, g, b):
    xf = x.astype(jnp.float32)
    mu = jnp.mean(xf, axis=-1, keepdims=True)
    var = jnp.mean(jnp.square(xf - mu), axis=-1, keepdims=True)
    y = (xf - mu) * lax.rsqrt(var + LN_EPS) * g.astype(jnp.float32) + b.astype(jnp.float32)
    return y.astype(x.dtype)


def _alibi_slopes():
    return jnp.asarray(2.0 ** (-8.0 * (np.arange(ATT_HEADS) + 1) / ATT_HEADS), dtype=jnp.float32)


def _sliding_window_attention(q, k, v, sinks):
    B, S = q.shape[0], q.shape[1]
    nb = S // ATT_BLOCK
    qb = q.reshape(B, nb, ATT_BLOCK, ATT_KV_HEADS, ATT_GROUP, ATT_HEAD_DIM)

    def band(t):
        tp = jnp.pad(t, ((0, 0), (ATT_BLOCK, 0), (0, 0), (0, 0)))
        tp = tp.reshape(B, nb + 1, ATT_BLOCK, ATT_KV_HEADS, ATT_HEAD_DIM)
        return jnp.concatenate([tp[:, :-1], tp[:, 1:]], axis=2)

    kb, vb = band(k), band(v)
    s = jnp.einsum('bnikgd,bnjkd->bnkgij', qb, kb, preferred_element_type=jnp.float32) * (ATT_HEAD_DIM ** -0.5)
    qi = jnp.arange(ATT_BLOCK)[:, None]
    kj = jnp.arange(2 * ATT_BLOCK)[None, :]
    dist = ATT_BLOCK + qi - kj
    key_pos = (jnp.arange(nb)[:, None, None] - 1) * ATT_BLOCK + kj[None]
    valid = (dist >= 0) & (dist < WINDOW) & (key_pos >= 0)
    slopes = _alibi_slopes().reshape(ATT_KV_HEADS, ATT_GROUP, 1, 1)
    s = s - slopes * dist.astype(jnp.float32)
    s = jnp.where(valid[None, :, None, None], s, -jnp.inf)
    sink = sinks.astype(jnp.float32).reshape(1, 1, ATT_KV_HEADS, ATT_GROUP, 1, 1)
    m = jnp.maximum(jnp.max(s, axis=-1, keepdims=True), sink)
    e = jnp.exp(s - m)
    probs = e / (jnp.sum(e, axis=-1, keepdims=True) + jnp.exp(sink - m))
    o = jnp.einsum('bnkgij,bnjkd->bnikgd', probs.astype(v.dtype), vb)
    return o.reshape(B, S, ATT_Q_WIDTH)


def _retention(q, k, v):
    B, S = q.shape[0], q.shape[1]
    nc = S // RET_CHUNK
    log_gamma = jnp.log(1.0 - 2.0 ** (-5.0 - jnp.arange(RET_HEADS, dtype=jnp.float32)))
    pos = jnp.arange(RET_CHUNK, dtype=jnp.float32)
    diff = pos[:, None] - pos[None, :]
    inner_decay = jnp.where(diff >= 0, jnp.exp(jnp.maximum(diff, 0.0) * log_gamma[:, None, None]), 0.0)
    q_decay = jnp.exp((pos + 1.0) * log_gamma[:, None])[None, :, :, None]
    k_decay = jnp.exp((RET_CHUNK - 1.0 - pos) * log_gamma[:, None])[None, :, :, None]
    chunk_decay = jnp.exp(RET_CHUNK * log_gamma)[None, :, None, None]

    def to_chunks(t):
        return t.astype(jnp.float32).reshape(B, nc, RET_CHUNK, RET_HEADS, t.shape[-1]).transpose(1, 0, 3, 2, 4)

    qc, kc, vc = to_chunks(q), to_chunks(k) * (RET_KEY_DIM ** -0.5), to_chunks(v)

    def step(state, chunk):
        qi, ki, vi = chunk
        inner = jnp.einsum('bhij,bhje->bhie', jnp.einsum('bhid,bhjd->bhij', qi, ki) * inner_decay, vi)
        cross = jnp.einsum('bhid,bhde->bhie', qi, state) * q_decay
        state = state * chunk_decay + jnp.einsum('bhjd,bhje->bhde', ki * k_decay, vi)
        return state, inner + cross

    state0 = jnp.zeros((B, RET_HEADS, RET_KEY_DIM, RET_VAL_DIM), jnp.float32)
    _, out = lax.scan(step, state0, (qc, kc, vc))
    return out.transpose(1, 0, 3, 2, 4).reshape(B, S, RET_HEADS, RET_VAL_DIM)


def _retention_branch(rq, rk, rv, rg, norm_g):
    B, S = rq.shape[0], rq.shape[1]
    o = _retention(rq.reshape(B, S, RET_HEADS, RET_KEY_DIM), rk.reshape(B, S, RET_HEADS, RET_KEY_DIM),
                   rv.reshape(B, S, RET_HEADS, RET_VAL_DIM))
    mu = jnp.mean(o, axis=-1, keepdims=True)
    var = jnp.mean(jnp.square(o - mu), axis=-1, keepdims=True)
    o = ((o - mu) * lax.rsqrt(var + LN_EPS)).reshape(B, S, RET_V_WIDTH) * norm_g.astype(jnp.float32)
    return (jax.nn.silu(rg.astype(jnp.float32)) * o).astype(rq.dtype)


def _clamped_swiglu(gu):
    glu, lin = gu[..., :EXPERT_FF], gu[..., EXPERT_FF:]
    glu = jnp.minimum(glu, SWIGLU_LIMIT)
    lin = jnp.clip(lin, -SWIGLU_LIMIT, SWIGLU_LIMIT)
    return glu * jax.nn.sigmoid(SWIGLU_ALPHA * glu) * (lin + 1.0)


def _moe(h, w_router, b_router, w_gate_up, b_gate_up, w_down, b_down):
    T = h.shape[0]
    TK = T * TOP_K
    nb = -(-TK // EXPERT_BLOCK) + N_EXPERTS
    logits = jnp.dot(h, w_router, preferred_element_type=jnp.float32) + b_router.astype(jnp.float32)
    top_vals, top_idx = lax.top_k(logits, TOP_K)
    gates = jax.nn.softmax(top_vals, axis=-1).reshape(TK)
    flat_e = top_idx.reshape(TK).astype(jnp.int32)
    order = jnp.argsort(flat_e)
    sorted_e = flat_e[order]
    counts = jnp.bincount(flat_e, length=N_EXPERTS).astype(jnp.int32)
    padded = (counts + EXPERT_BLOCK - 1) // EXPERT_BLOCK * EXPERT_BLOCK
    group_start = jnp.cumsum(counts) - counts
    padded_end = jnp.cumsum(padded)
    padded_start = padded_end - padded
    dest = padded_start[sorted_e] + jnp.arange(TK, dtype=jnp.int32) - group_start[sorted_e]
    n_rows = nb * EXPERT_BLOCK
    row_token = jnp.zeros((n_rows,), jnp.int32).at[dest].set((order // TOP_K).astype(jnp.int32))
    row_gate = jnp.zeros((n_rows,), jnp.float32).at[dest].set(gates[order])
    block_start = jnp.arange(nb, dtype=jnp.int32) * EXPERT_BLOCK
    block_expert = jnp.minimum(jnp.searchsorted(padded_end, block_start, side='right'), N_EXPERTS - 1).astype(jnp.int32)

    def block_step(acc, blk):
        e, rows, g = blk
        xb = h[rows]
        gu = xb @ w_gate_up[e] + b_gate_up[e]
        y = _clamped_swiglu(gu) @ w_down[e] + b_down[e]
        return acc.at[rows].add(y * g[:, None].astype(y.dtype)), None

    out, _ = lax.scan(block_step, jnp.zeros_like(h),
                      (block_expert, row_token.reshape(nb, EXPERT_BLOCK), row_gate.reshape(nb, EXPERT_BLOCK)))
    return out


def setup_inputs(seed: int = 0) -> dict:
    key = jax.random.key(seed)
    ks = jax.random.split(key, 22)
    f32 = jnp.float32
    L = DEPTH
    beta = DEEPNORM_BETA

    def nrm(k, shape, scale):
        return jax.random.normal(k, shape, f32) * scale

    return {
        'x': nrm(ks[0], (BATCH, SEQ, D_MODEL), 1.0),
        'p': nrm(ks[1], (DEPTH, BATCH, SEQ, PLE_DIM), 1.0),
        'w_in': nrm(ks[2], (L, D_MODEL, IN_WIDTH), D_MODEL ** -0.5),
        'attn_sinks': nrm(ks[3], (L, ATT_HEADS), 1.0),
        'ret_norm_g': 1.0 + nrm(ks[4], (L, RET_V_WIDTH), 0.02),
        'w_att_out': nrm(ks[5], (L, ATT_Q_WIDTH, D_MODEL), ATT_Q_WIDTH ** -0.5),
        'w_ret_out': nrm(ks[6], (L, RET_V_WIDTH, D_MODEL), RET_V_WIDTH ** -0.5),
        'w_out': nrm(ks[7], (L, D_MODEL, D_MODEL), D_MODEL ** -0.5 * beta),
        'ln1_g': 1.0 + nrm(ks[8], (L, D_MODEL), 0.02),
        'ln1_b': nrm(ks[9], (L, D_MODEL), 0.02),
        'w_router': nrm(ks[10], (L, D_MODEL, N_EXPERTS), D_MODEL ** -0.5),
        'b_router': nrm(ks[11], (L, N_EXPERTS), 0.01),
        'w_gate_up': nrm(ks[12], (L, N_EXPERTS, D_MODEL, 2 * EXPERT_FF), D_MODEL ** -0.5),
        'b_gate_up': nrm(ks[13], (L, N_EXPERTS, 2 * EXPERT_FF), 0.01),
        'w_down': nrm(ks[14], (L, N_EXPERTS, EXPERT_FF, D_MODEL), EXPERT_FF ** -0.5 * beta),
        'b_down': nrm(ks[15], (L, N_EXPERTS, D_MODEL), 0.01),
        'ln2_g': 1.0 + nrm(ks[16], (L, D_MODEL), 0.02),
        'ln2_b': nrm(ks[17], (L, D_MODEL), 0.02),
        'w_ple': nrm(ks[18], (L, PLE_DIM, D_MODEL), PLE_DIM ** -0.5 * beta),
        'w_ple_gate': nrm(ks[19], (L, D_MODEL, D_MODEL), D_MODEL ** -0.5),
        'ln3_g': 1.0 + nrm(ks[20], (L, D_MODEL), 0.02),
        'ln3_b': nrm(ks[21], (L, D_MODEL), 0.02),
    }


def reference(x, p, w_in, attn_sinks, ret_norm_g, w_att_out, w_ret_out, w_out, ln1_g, ln1_b,
              w_router, b_router, w_gate_up, b_gate_up, w_down, b_down, ln2_g, ln2_b,
              w_ple, w_ple_gate, ln3_g, ln3_b):
    B, S, D = x.shape
    for i in range(DEPTH):
        proj = x @ w_in[i]
        aq, ak, av, rq, rk, rv, rg, ga, gr = jnp.split(proj, IN_OFFSETS, axis=-1)
        y_att = _sliding_window_attention(aq.reshape(B, S, ATT_HEADS, ATT_HEAD_DIM),
                                          ak.reshape(B, S, ATT_KV_HEADS, ATT_HEAD_DIM),
                                          av.reshape(B, S, ATT_KV_HEADS, ATT_HEAD_DIM), attn_sinks[i])
        y_ret = _retention_branch(rq, rk, rv, rg, ret_norm_g[i])
        merged = jax.nn.sigmoid(ga) * (y_att @ w_att_out[i]) + jax.nn.sigmoid(gr) * (y_ret @ w_ret_out[i])
        x = _layer_norm(DEEPNORM_ALPHA * x + merged @ w_out[i], ln1_g[i], ln1_b[i])
        moe_out = _moe(x.reshape(B * S, D), w_router[i], b_router[i], w_gate_up[i], b_gate_up[i],
                       w_down[i], b_down[i]).reshape(B, S, D)
        x = _layer_norm(DEEPNORM_ALPHA * x + moe_out, ln2_g[i], ln2_b[i])
        ple = (p[i] @ w_ple[i]) * jax.nn.sigmoid(x @ w_ple_gate[i])
        x = _layer_norm(DEEPNORM_ALPHA * x + ple, ln3_g[i], ln3_b[i])
    return x
```

```python
import math
from contextlib import ExitStack
import numpy as np
import concourse.bass as bass
import concourse.mybir as mybir
from concourse.bass_utils import run_bass_kernel_spmd

F32 = mybir.dt.float32
BF16 = mybir.dt.bfloat16
ALU = mybir.AluOpType
AF = mybir.ActivationFunctionType
AX = mybir.AxisListType

FULL = dict(D=4096, AH=32, AKV=4, RH=8, NE=32, FF=1536, PLE=256, TOK=2048, NPREV=6144,
            SEQ=8192, BATCH=2, NCORE=8, CAP=384)
HD, DK, DV = 64, 128, 256
TOPK = 4
LN_EPS = 1e-5
LIMIT = 7.0
SW_ALPHA = 1.702
T = 512
NT = 4
PW = 256


class TB:
    def __init__(self, t, name):
        self.t = t
        self.name = name
        self.w = {}
        self.r = {}

    def __getitem__(self, k):
        return self.t[k]


class Sync:
    NDMA = 20

    def __init__(self, nc, es):
        self.nc = nc
        self.e = {}
        for nm, attr in (("pe", "tensor"), ("dve", "vector"), ("act", "scalar"), ("pool", "gpsimd"), ("sp", "sync")):
            sem = es.enter_context(nc.semaphore("s_" + nm))
            self.e[nm] = dict(eng=getattr(nc, attr), sem=sem, cnt=0, seen={})
        self.dq = {}
        for q in ("pool", "sp"):
            sems = [es.enter_context(nc.semaphore(f"d_{q}{i}")) for i in range(self.NDMA)]
            self.dq[q] = dict(sems=sems, val=[0] * self.NDMA, rr=0)

    def _wait(self, en, tok):
        sem, val, key = tok
        e = self.e[en]
        if e["seen"].get(key, 0) < val:
            e["eng"].wait_ge(sem, val)
            e["seen"][key] = val

    def _deps(self, en, reads, writes, skip_same):
        for b in reads:
            for key, tok in b.w.items():
                if skip_same and key == en:
                    continue
                self._wait(en, tok)
        for b in writes:
            for d in (b.w, b.r):
                for key, tok in d.items():
                    if skip_same and key == en:
                        continue
                    self._wait(en, tok)

    def _mark(self, tok, reads, writes):
        key = tok[2]
        for b in reads:
            b.r[key] = tok
        for b in writes:
            b.w[key] = tok

    def op(self, en, fn, reads=(), writes=()):
        e = self.e[en]
        self._deps(en, reads, writes, skip_same=(en == "pe"))
        inst = fn(e["eng"])
        e["cnt"] += 1
        inst.then_inc(e["sem"], 1)
        self._mark((e["sem"], e["cnt"], en), reads, writes)

    def dma(self, q, out, in_, reads=(), writes=(), **kw):
        self._deps(q, reads, writes, skip_same=False)
        d = self.dq[q]
        i = d["rr"]
        d["rr"] = (i + 1) % self.NDMA
        key = (q, i)
        if d["val"][i] > 0:
            self._wait(q, (d["sems"][i], d["val"][i], key))
        inst = self.e[q]["eng"].dma_start(out=out, in_=in_, **kw)
        d["val"][i] += 16
        inst.then_inc(d["sems"][i], 16)
        self._mark((d["sems"][i], d["val"][i], key), reads, writes)

    def idma(self, out, out_off, in_, in_off, reads=(), writes=(), **kw):
        q = "pool"
        self._deps(q, reads, writes, skip_same=False)
        d = self.dq[q]
        i = d["rr"]
        d["rr"] = (i + 1) % self.NDMA
        key = (q, i)
        if d["val"][i] > 0:
            self._wait(q, (d["sems"][i], d["val"][i], key))
        inst = self.e[q]["eng"].indirect_dma_start(out=out, out_offset=out_off, in_=in_, in_offset=in_off, **kw)
        d["val"][i] += 16
        inst.then_inc(d["sems"][i], 16)
        self._mark((d["sems"][i], d["val"][i], key), reads, writes)

    def barrier(self):
        toks = []
        for nm, e in self.e.items():
            if e["cnt"] > 0:
                toks.append((e["sem"], e["cnt"], nm))
        for q, d in self.dq.items():
            for i in range(self.NDMA):
                if d["val"][i] > 0:
                    toks.append((d["sems"][i], d["val"][i], (q, i)))
        for nm in self.e:
            for tok in toks:
                if tok[2] != nm:
                    self._wait(nm, tok)


def build(cfg):
    D, AH, AKV, RH, NE, FF, PLE = cfg["D"], cfg["AH"], cfg["AKV"], cfg["RH"], cfg["NE"], cfg["FF"], cfg["PLE"]
    TOK, NPREV = cfg["TOK"], cfg["NPREV"]
    G = AH // AKV
    KC = D // 128
    KG = min(16, KC)
    AQW, AKW, RQW, RVW = AH * HD, AKV * HD, RH * DK, RH * DV
    o_aq = 0
    o_ak = o_aq + AQW
    o_av = o_ak + AKW
    o_rq = o_av + AKW
    o_rk = o_rq + RQW
    o_rv = o_rk + RQW
    o_rg = o_rv + RVW
    o_ga = o_rg + RVW
    o_gr = o_ga + D
    INW = o_gr + D
    NST = TOK // T
    NPST = NPREV // T
    NPP = FF // PW
    FKC = FF // 128
    alpha = float((2 * 1) ** 0.25)
    gam = [1.0 - 2.0 ** (-5.0 - h) for h in range(RH)]
    g128 = [float(np.float32(g ** 128)) for g in gam]

    nc = bass.Bass("TRN2", target_bir_lowering=False)

    def din(name, shape):
        return nc.dram_tensor(name, list(shape), F32, kind="ExternalInput").ap()

    x_d = din("x", [TOK, D])
    xp_d = din("xprev", [NPREV, D])
    p_d = din("p", [TOK, PLE])
    w_in = din("w_in", [D, INW])
    sinks_d = din("attn_sinks", [1, AH])
    rng_d = din("ret_norm_g", [1, RVW])
    w_ao = din("w_att_out", [AQW, D])
    w_ro = din("w_ret_out", [RVW, D])
    w_out = din("w_out", [D, D])
    ln_d = [(din(f"ln{i}_g", [1, D]), din(f"ln{i}_b", [1, D])) for i in (1, 2, 3)]
    w_rt = din("w_router", [D, NE])
    b_rt = din("b_router", [1, NE])
    w_gu = din("w_gate_up", [NE, D, 2 * FF])
    b_gu = din("b_gate_up", [NE, 2 * FF])
    w_dn = din("w_down", [NE, FF, D])
    b_dn = din("b_down", [NE, D])
    w_ple = din("w_ple", [PLE, D])
    w_pg = din("w_ple_gate", [D, D])
    c_ident = din("c_ident", [128, 128])
    c_tabA = din("c_tabA", [128, 2 * AH * 128])
    c_tabF = din("c_tabF", [128, AH * 128])
    c_DT = din("c_DT", [128, RH * 128])
    c_qdec = din("c_qdec", [128, RH * 128])
    c_kdec = din("c_kdec", [128, RH])
    CAP = cfg["CAP"]
    c_utri = din("c_utri", [128, 128])
    c_iota = din("c_iota", [128, CAP])
    c_dummy = din("c_dummy", [128, CAP // 128])
    c_tokid = din("c_tokid", [128, TOK // 128])
    out_d = nc.dram_tensor("out", [TOK, D], F32, kind="ExternalOutput").ap()

    def scratch(name, shape, dt):
        return TB(nc.dram_tensor(name, list(shape), dt, kind="Internal").ap(), name)

    yatt_d = scratch("yatt_d", [TOK, AQW], BF16)
    yret_d = scratch("yret_d", [TOK, RVW], BF16)
    sga_d = scratch("sga_d", [TOK, D], F32)
    sgr_d = scratch("sgr_d", [TOK, D], F32)
    mrg_d = scratch("mrg_d", [TOK, D], BF16)
    srg_d = scratch("srg_d", [TOK, RVW], BF16)
    x1_d = scratch("x1_d", [TOK, D], F32)
    x1b_d = scratch("x1b_d", [TOK + cfg["CAP"], D], BF16)
    RCH = min(2048, D)
    acc_ds = [scratch(f"acc_d{c}", [TOK + cfg["CAP"], RCH], F32) for c in range(D // RCH)]
    x2_d = scratch("x2_d", [TOK, D], F32)
    x2b_d = scratch("x2b_d", [TOK, D], BF16)
    out_tb = TB(out_d, "out")

    with ExitStack() as es0:
        S = Sync(nc, es0)

        uniq = [0]

        def sb(es, name, shape, dt):
            uniq[0] += 1
            return TB(es.enter_context(nc.sbuf_tensor(f"{name}_{uniq[0]}", list(shape), dt)), name)

        PS = [TB(es0.enter_context(nc.psum_tensor(f"ps{i}", [128, 512], F32)), f"ps{i}") for i in range(8)]
        ident = sb(es0, "ident", [128, 128], BF16)
        ones = sb(es0, "ones", [1, 128], BF16)
        S.dma("pool", ident[:, :], c_ident[:, :], writes=[ident])
        S.op("dve", lambda e: e.memset(ones[:, :], 1.0), writes=[ones])
        evac_rr = [0]

        def evac(out_ap, in_ap, reads, writes):
            evac_rr[0] ^= 1
            if evac_rr[0]:
                S.op("act", lambda e: e.copy(out=out_ap, in_=in_ap), reads, writes)
            else:
                S.op("dve", lambda e: e.tensor_copy(out=out_ap, in_=in_ap), reads, writes)

        def load_actT(es_stage, src_ap_fn, K, actT, src_tb, tps=(6, 7), stage=None, nt=NT, q="pool"):
            kc_n = K // 128
            for t in range(nt):
                stg = stage[t % len(stage)]
                S.dma(q, stg[:, 0:K], src_ap_fn(t), reads=[src_tb] if src_tb else [], writes=[stg])
                for k0 in range(0, kc_n, 4):
                    kn = min(4, kc_n - k0)
                    ps = PS[tps[(k0 // 4) % 2]]

                    def fn(e, k0=k0, kn=kn, ps=ps, stg=stg):
                        for j in range(kn):
                            inst = e.matmul(ps[:, j * 128:(j + 1) * 128], stg[:, (k0 + j) * 128:(k0 + j + 1) * 128],
                                            ident[:, :], start=True, stop=True)
                        return inst
                    S.op("pe", fn, reads=[stg, ident], writes=[ps])
                    evac(actT[:, k0:k0 + kn, t * 128:(t + 1) * 128],
                         ps[:, 0:kn * 128].rearrange("p (a b) -> p a b", b=128), [ps], [actT])

        def wtile_kp(w_ap2d, k0, nk, c0, cn):
            return w_ap2d.rearrange("(kc p) n -> p kc n", p=128)[:, k0:k0 + nk, c0:c0 + cn]

        def gemm(actT, kc_n, w_ap2d, panels, wring, epi_B=None, epi_A=None, bias_fn=None, tiles=range(NT), psB=(0, 1, 2, 3), psA=(4, 5)):
            kg = min(KG, kc_n)
            ngrp = kc_n // kg
            for (c0, cn, kind, M) in panels:
                wts = []
                for g in range(ngrp):
                    wt = wring[wring_rr[0] % len(wring)]
                    wring_rr[0] += 1
                    S.dma("pool", wt[:, 0:kg, 0:cn], wtile_kp(w_ap2d, g * kg, kg, c0, cn), writes=[wt])
                    wts.append(wt)
                if kind == "B":
                    for g in range(ngrp):
                        wt = wts[g]
                        for t in tiles:
                            ps = PS[psB[t % len(psB)]]

                            def fn(e, g=g, wt=wt, t=t, ps=ps):
                                for k in range(kg):
                                    inst = e.matmul(ps[:, 0:cn], actT[:, g * kg + k, t * 128:(t + 1) * 128], wt[:, k, 0:cn],
                                                    start=(g == 0 and k == 0), stop=(g == ngrp - 1 and k == kg - 1))
                                return inst
                            S.op("pe", fn, reads=[actT, wt], writes=[ps])
                            if g == ngrp - 1:
                                epi_B(t, c0, cn, ps)
                else:
                    nblk = cn // M
                    for blk in range(nblk):
                        ps = PS[psA[blk % len(psA)]]

                        def fn(e, blk=blk, ps=ps):
                            for g in range(ngrp):
                                for k in range(kg):
                                    inst = e.matmul(ps[0:M, 0:T], wts[g][:, k, blk * M:(blk + 1) * M], actT[:, g * kg + k, 0:T],
                                                    start=(g == 0 and k == 0), stop=(g == ngrp - 1 and k == kg - 1))
                            return inst
                        S.op("pe", fn, reads=[actT] + wts, writes=[ps])
                        epi_A(c0 + blk * M, M, ps)

        wring_rr = [0]

        def layer_norm(es, z, zt_ap, g_bc, b_bc, junk, out32_ap, out32_tb, outbf_ap=None, outbf_tb=None, zb=None):
            st = ln_small
            S.op("dve", lambda e: e.reduce_sum(out=st[:, 0:1], in_=zt_ap, axis=AX.X), [z], [st])
            S.op("dve", lambda e: e.tensor_tensor(out=junk[:, 0:D], in0=zt_ap, in1=zt_ap, op=ALU.mult), [z], [junk])
            S.op("dve", lambda e: e.reduce_sum(out=st[:, 1:2], in_=junk[:, 0:D], axis=AX.X), [junk], [st])
            S.op("dve", lambda e: e.tensor_scalar(out=st[:, 2:4], in0=st[:, 0:2], scalar1=1.0 / D, scalar2=None, op0=ALU.mult), [st], [st])
            S.op("dve", lambda e: e.tensor_tensor(out=st[:, 4:5], in0=st[:, 2:3], in1=st[:, 2:3], op=ALU.mult), [st], [st])
            S.op("dve", lambda e: e.tensor_tensor(out=st[:, 5:6], in0=st[:, 3:4], in1=st[:, 4:5], op=ALU.subtract), [st], [st])
            S.op("dve", lambda e: e.tensor_scalar(out=st[:, 6:7], in0=st[:, 5:6], scalar1=LN_EPS, scalar2=None, op0=ALU.add), [st], [st])
            S.op("act", lambda e: e.sqrt(out=st[:, 6:7], in_=st[:, 6:7]), [st], [st])
            S.op("dve", lambda e: e.reciprocal(out=st[:, 6:7], in_=st[:, 6:7]), [st], [st])
            S.op("dve", lambda e: e.tensor_scalar(out=zt_ap, in0=zt_ap, scalar1=st[:, 2:3], scalar2=st[:, 6:7], op0=ALU.subtract, op1=ALU.mult), [z, st], [z])
            S.op("dve", lambda e: e.tensor_tensor(out=zt_ap, in0=zt_ap, in1=g_bc[:, :], op=ALU.mult), [z, g_bc], [z])
            S.op("dve", lambda e: e.tensor_tensor(out=zt_ap, in0=zt_ap, in1=b_bc[:, :], op=ALU.add), [z, b_bc], [z])
            S.dma("sp", out32_ap, zt_ap, reads=[z], writes=[out32_tb])
            if outbf_ap is not None:
                S.op("act", lambda e: e.copy(out=zb[:, 0:D], in_=zt_ap), [z], [zb])
                S.dma("sp", outbf_ap, zb[:, 0:D], reads=[zb], writes=[outbf_tb])

        ln_small = sb(es0, "ln_small", [128, 8], F32)

        def load_bc(es, name, src_ap, n):
            t_ = sb(es, name, [128, n], F32)
            S.dma("sp", t_[:, :], src_ap.partition_broadcast(128), writes=[t_])
            return t_

        with ExitStack() as esAB:
            xT = sb(esAB, "xT", [128, KC, T], BF16)
            stage = [sb(esAB, "stage0", [128, D], BF16)]
            wring = [sb(esAB, f"wr{i}", [128, KG, 512], BF16) for i in range(max(3, KC // KG))]
            kT = sb(esAB, "kT", [64, AKV, 128 + T], BF16)
            v1 = sb(esAB, "v1", [128, NT + 1, AKV, HD + 1], BF16)
            state = sb(esAB, "state", [128, RH, DV], F32)
            state_bf = sb(esAB, "state_bf", [128, RH, DV], BF16)
            kdec = sb(esAB, "kdec", [128, RH], F32)
            S.dma("sp", kdec[:, :], c_kdec[:, :], writes=[kdec])
            S.op("dve", lambda e: e.memset(v1[:, :, :, :], 1.0), writes=[v1])
            S.op("dve", lambda e: e.memset(state[:, :, :], 0.0), writes=[state])
            S.op("dve", lambda e: e.memset(state_bf[:, :, :], 0.0), writes=[state_bf])

            def kv_halo_epis():
                def epi_A(c, M, ps):
                    kvh = (c - o_ak) // HD
                    evac(kT[:, kvh, 0:128], ps[0:HD, T - 128:T], [ps], [kT])

                def epi_B(t, c0, cn, ps):
                    if t == NT - 1:
                        evac(v1[:, 0, :, 0:HD], ps[:, 0:AKW].rearrange("p (a b) -> p a b", b=HD), [ps], [v1])
                return epi_A, epi_B

            def ret_state_update(ksc, vsb, t, ps_banks=(6, 7)):
                for h0 in range(0, RH, 2):
                    ps = PS[ps_banks[(h0 // 2) % 2]]

                    def fn(e, h0=h0, ps=ps):
                        for j in range(2):
                            h = h0 + j
                            inst = e.matmul(ps[:, j * DV:(j + 1) * DV], ksc[:, t, h * DK:(h + 1) * DK], vsb[:, t, h * DV:(h + 1) * DV],
                                            start=True, stop=True)
                        return inst
                    S.op("pe", fn, reads=[ksc, vsb], writes=[ps])
                    for j in range(2):
                        h = h0 + j
                        S.op("dve", lambda e, h=h, j=j, ps=ps: e.scalar_tensor_tensor(
                            out=state[:, h, :], in0=state[:, h, :], scalar=g128[h], in1=ps[:, j * DV:(j + 1) * DV],
                            op0=ALU.mult, op1=ALU.add), [state, ps], [state])
                S.op("act", lambda e: e.copy(out=state_bf[:, :, :], in_=state[:, :, :]), [state], [state_bf])

            with ExitStack() as esA:
                ksc = sb(esA, "ksc", [128, NT, RQW], BF16)
                vsb = sb(esA, "vsb", [128, NT, RVW], BF16)
                for pst in range(NPST):
                    load_actT(esA, lambda t, pst=pst: xp_d[pst * T + t * 128: pst * T + (t + 1) * 128, :], D, xT, None, stage=stage)

                    def epi_B(t, c0, cn, ps):
                        if c0 < o_rv:
                            for hh in range(cn // DK):
                                h = (c0 - o_rk) // DK + hh
                                S.op("dve", lambda e, h=h, hh=hh: e.tensor_scalar(
                                    out=ksc[:, t, h * DK:(h + 1) * DK], in0=ps[:, hh * DK:(hh + 1) * DK], scalar1=kdec[:, h:h + 1],
                                    scalar2=None, op0=ALU.mult), [ps, kdec], [ksc])
                        else:
                            evac(vsb[:, t, c0 - o_rv:c0 - o_rv + cn], ps[:, 0:cn], [ps], [vsb])
                    panels = [(c, min(512, o_rg - c), "B", 0) for c in range(o_rk, o_rg, 512)]
                    panels = []
                    for c in range(o_rk, o_rv, 512):
                        panels.append((c, min(512, o_rv - c), "B", 0))
                    for c in range(o_rv, o_rg, 512):
                        panels.append((c, min(512, o_rg - c), "B", 0))
                    gemm(xT, KC, w_in, panels, wring, epi_B=epi_B)
                    if pst == NPST - 1:
                        eA, eB = kv_halo_epis()
                        gemm(xT, KC, w_in, [(o_ak, AKW, "A", HD)], wring, epi_A=eA)
                        gemm(xT, KC, w_in, [(o_av, AKW, "B", 0)], wring, epi_B=eB, tiles=[NT - 1])
                    for t in range(NT):
                        ret_state_update(ksc, vsb, t)
                S.barrier()

            for st in range(NST):
                tok0 = st * T
                load_actT(esAB, lambda t: x_d[tok0 + t * 128: tok0 + (t + 1) * 128, :], D, xT, None, stage=stage)
                with ExitStack() as esI:
                    qT = sb(esI, "qT", [64, AH, T], BF16)
                    tabA = sb(esI, "tabA", [128, 2, AH, 128], BF16)
                    tabF = sb(esI, "tabF", [128, AH, 128], BF16)
                    esink = sb(esI, "esink", [128, AH], F32)
                    e_sb = [sb(esI, f"e_sb{i}", [128, 512], BF16) for i in range(2)]
                    PT = [sb(esI, f"PT{i}", [128, 4, 128], BF16) for i in range(2)]
                    den = sb(esI, "den", [128, 8], F32)
                    yatt = [sb(esI, f"yatt{i}", [128, AQW], BF16) for i in range(2)]
                    S.dma("pool", tabA[:, :, :, :], c_tabA.rearrange("p (c h i) -> p c h i", c=2, h=AH), writes=[tabA])
                    S.dma("pool", tabF[:, :, :], c_tabF.rearrange("p (h i) -> p h i", h=AH), writes=[tabF])
                    S.dma("sp", esink[:, :], sinks_d.partition_broadcast(128), writes=[esink])
                    S.op("act", lambda e: e.activation(out=esink[:, :], in_=esink[:, :], func=AF.Exp), [esink], [esink])

                    def epi_Aq(c, M, ps):
                        if c < o_ak:
                            evac(qT[:, c // HD, :], ps[0:HD, 0:T], [ps], [qT])
                        else:
                            kvh = (c - o_ak) // HD
                            evac(kT[:, kvh, 128:128 + T], ps[0:HD, 0:T], [ps], [kT])

                    def epi_Bv(t, c0, cn, ps):
                        evac(v1[:, t + 1, :, 0:HD], ps[:, 0:AKW].rearrange("p (a b) -> p a b", b=HD), [ps], [v1])
                    panels = [(c, min(512, AQW - c), "A", HD) for c in range(0, AQW, 512)] + [(o_ak, AKW, "A", HD)]
                    gemm(xT, KC, w_in, panels, wring, epi_A=epi_Aq)
                    gemm(xT, KC, w_in, [(o_av, AKW, "B", 0)], wring, epi_B=epi_Bv)
                    for t in range(NT):
                        ya = yatt[t % 2]
                        for kvh in range(AKV):
                            for h0 in range(kvh * G, (kvh + 1) * G, 4):
                                nh = min(4, G)
                                for c in range(2):
                                    ps = PS[c]
                                    S.op("pe", lambda e, c=c, ps=ps, h0=h0, kvh=kvh, t=t: e.matmul(
                                        ps[:, 0:nh * 128], kT[:, kvh, (t + c) * 128:(t + c + 1) * 128],
                                        qT[:, h0:h0 + nh, t * 128:(t + 1) * 128], start=True, stop=True), [kT, qT], [ps])
                                    S.op("act", lambda e, c=c, ps=ps: e.activation(out=e_sb[c][:, 0:nh * 128], in_=ps[:, 0:nh * 128],
                                                                                   func=AF.Exp, scale=HD ** -0.5), [ps], [e_sb[c]])
                                    tab = tabF[:, h0:h0 + nh, :] if (st == 0 and t == 0 and c == 0) else tabA[:, c, h0:h0 + nh, :]
                                    S.op("dve", lambda e, c=c, tab=tab: e.tensor_tensor(
                                        out=PT[c][:, 0:nh, :], in0=e_sb[c][:, 0:nh * 128].rearrange("p (a b) -> p a b", b=128), in1=tab,
                                        op=ALU.mult), [e_sb[c], tabA, tabF], [PT[c]])
                                pso = PS[2 + ((h0 // 4) % 2)]

                                def fn(e, pso=pso, kvh=kvh, t=t):
                                    for hh in range(nh):
                                        e.matmul(pso[:, hh * 128:hh * 128 + HD + 1], PT[0][:, hh, :], v1[:, t, kvh, :], start=True, stop=False)
                                        inst = e.matmul(pso[:, hh * 128:hh * 128 + HD + 1], PT[1][:, hh, :], v1[:, t + 1, kvh, :], start=False, stop=True)
                                    return inst
                                S.op("pe", fn, reads=[PT[0], PT[1], v1], writes=[pso])
                                pv = pso[:, 0:nh * 128].rearrange("p (a b) -> p a b", b=128)
                                S.op("dve", lambda e, pv=pv, h0=h0: e.tensor_tensor(out=den[:, 0:nh], in0=pv[:, :, HD], in1=esink[:, h0:h0 + nh], op=ALU.add),
                                     [pso, esink], [den])
                                S.op("dve", lambda e: e.reciprocal(out=den[:, 4:4 + nh], in_=den[:, 0:nh]), [den], [den])
                                for hh in range(nh):
                                    h = h0 + hh
                                    S.op("act", lambda e, hh=hh, h=h, pso=pso, ya=ya: e.activation(
                                        out=ya[:, h * HD:(h + 1) * HD], in_=pso[:, hh * 128:hh * 128 + HD], func=AF.Copy,
                                        scale=den[:, 4 + hh:5 + hh]), [pso, den], [ya])
                        S.dma("sp", yatt_d[tok0 + t * 128: tok0 + (t + 1) * 128, :], ya[:, :], reads=[ya], writes=[yatt_d])
                    S.op("act", lambda e: e.copy(out=kT[:, :, 0:128], in_=kT[:, :, T:T + 128]), [kT], [kT])
                    S.op("dve", lambda e: e.tensor_copy(out=v1[:, 0, :, :], in_=v1[:, NT, :, :]), [v1], [v1])
                    S.barrier()
                with ExitStack() as esII:
                    rqT = sb(esII, "rqT", [128, RH, T], BF16)
                    rkT = sb(esII, "rkT", [128, RH, T], BF16)
                    ksc = sb(esII, "ksc2", [128, NT, RQW], BF16)
                    vsb = sb(esII, "vsb2", [128, NT, RVW], BF16)
                    srg = [sb(esII, f"srg{i}", [128, RVW], BF16) for i in range(2)]
                    gsb = [sb(esII, f"gsb{i}", [128, 512], BF16) for i in range(2)]
                    DT = sb(esII, "DT", [128, RH, 128], F32)
                    qdec = sb(esII, "qdec", [128, RH, 128], BF16)
                    normg = load_bc(esII, "normg", rng_d, RVW)
                    AT = sb(esII, "AT", [128, RH, 128], BF16)
                    rqs = sb(esII, "rqs", [128, RH, 128], BF16)
                    o_sb = sb(esII, "o_sb", [128, RH, DV], F32)
                    junk = stage[0]
                    rst = sb(esII, "rst", [128, 6, RH], F32)
                    yret = [sb(esII, "yret0", [128, RVW], BF16)]
                    gst = [sb(esII, f"gst{i}", [128, 512], F32) for i in range(2)]
                    S.dma("sp", DT[:, :, :], c_DT.rearrange("p (h i) -> p h i", h=RH), writes=[DT])
                    S.dma("pool", qdec[:, :, :], c_qdec.rearrange("p (h i) -> p h i", h=RH), writes=[qdec])

                    def epi_Ar(c, M, ps):
                        if c < o_rk:
                            evac(rqT[:, (c - o_rq) // DK, :], ps[:, 0:T], [ps], [rqT])
                        else:
                            evac(rkT[:, (c - o_rk) // DK, :], ps[:, 0:T], [ps], [rkT])
                    gst_rr = [0]

                    def epi_Br(t, c0, cn, ps):
                        if c0 < o_rv:
                            for hh in range(cn // DK):
                                h = (c0 - o_rk) // DK + hh
                                S.op("dve", lambda e, h=h, hh=hh: e.tensor_scalar(
                                    out=ksc[:, t, h * DK:(h + 1) * DK], in0=ps[:, hh * DK:(hh + 1) * DK], scalar1=kdec[:, h:h + 1],
                                    scalar2=None, op0=ALU.mult), [ps, kdec], [ksc])
                        elif c0 < o_rg:
                            evac(vsb[:, t, c0 - o_rv:c0 - o_rv + cn], ps[:, 0:cn], [ps], [vsb])
                        elif c0 < o_ga:
                            g_ = gsb[gst_rr[0] % 2]
                            gst_rr[0] += 1
                            S.op("act", lambda e: e.activation(out=g_[:, 0:cn], in_=ps[:, 0:cn], func=AF.Silu), [ps], [g_])
                            S.dma("sp", srg_d[tok0 + t * 128: tok0 + (t + 1) * 128, c0 - o_rg:c0 - o_rg + cn], g_[:, 0:cn], reads=[g_], writes=[srg_d])
                        else:
                            g_ = gst[gst_rr[0] % 2]
                            gst_rr[0] += 1
                            S.op("act", lambda e: e.activation(out=g_[:, 0:cn], in_=ps[:, 0:cn], func=AF.Sigmoid), [ps], [g_])
                            dst, cc = (sga_d, c0 - o_ga) if c0 < o_gr else (sgr_d, c0 - o_gr)
                            S.dma("sp", dst[tok0 + t * 128: tok0 + (t + 1) * 128, cc:cc + cn], g_[:, 0:cn], reads=[g_], writes=[dst])
                    panels = [(c, min(512, o_rv - c), "A", DK) for c in range(o_rq, o_rv, 512)]
                    gemm(xT, KC, w_in, panels, wring, epi_A=epi_Ar)
                    panels = []
                    for lo, hi in ((o_rk, o_rv), (o_rv, o_rg), (o_rg, o_ga), (o_ga, o_gr), (o_gr, INW)):
                        for c in range(lo, hi, 512):
                            panels.append((c, min(512, hi - c), "B", 0))
                    gemm(xT, KC, w_in, panels, wring, epi_B=epi_Br)
                    for t in range(NT):
                        tsl = slice(t * 128, (t + 1) * 128)
                        for hg in range(0, RH, 4):
                            nh = min(4, RH - hg)
                            ps = PS[(hg // 4) % 2]

                            def fn(e, hg=hg, nh=nh, ps=ps):
                                for j in range(nh):
                                    inst = e.matmul(ps[:, j * 128:(j + 1) * 128], rkT[:, hg + j, tsl], rqT[:, hg + j, tsl], start=True, stop=True)
                                return inst
                            S.op("pe", fn, [rkT, rqT], [ps])
                            S.op("dve", lambda e, hg=hg, nh=nh, ps=ps: e.tensor_tensor(
                                out=AT[:, hg:hg + nh, :], in0=ps[:, 0:nh * 128].rearrange("p (a b) -> p a b", b=128), in1=DT[:, hg:hg + nh, :],
                                op=ALU.mult), [ps, DT], [AT])
                        S.op("dve", lambda e: e.tensor_tensor(out=rqs[:, :, :], in0=rqT[:, :, tsl], in1=qdec[:, :, :], op=ALU.mult), [rqT, qdec], [rqs])
                        for h0 in range(0, RH, 2):
                            ps = PS[2 + (h0 // 2) % 4]

                            def fn(e, h0=h0, ps=ps):
                                for j in range(2):
                                    h = h0 + j
                                    e.matmul(ps[:, j * DV:(j + 1) * DV], AT[:, h, :], vsb[:, t, h * DV:(h + 1) * DV], start=True, stop=False)
                                    inst = e.matmul(ps[:, j * DV:(j + 1) * DV], rqs[:, h, :], state_bf[:, h, :], start=False, stop=True)
                                return inst
                            S.op("pe", fn, [AT, vsb, rqs, state_bf], [ps])
                            evac(o_sb[:, h0:h0 + 2, :], ps[:, 0:2 * DV].rearrange("p (a b) -> p a b", b=DV), [ps], [o_sb])
                        ret_state_update(ksc, vsb, t)
                        S.op("dve", lambda e: e.tensor_reduce(out=rst[:, 0, :], in_=o_sb[:, :, :], axis=AX.X, op=ALU.add), [o_sb], [rst])
                        jv = junk[:, 0:RVW].rearrange("p (a b) -> p a b", b=DV)
                        S.op("dve", lambda e: e.tensor_tensor(out=jv, in0=o_sb[:, :, :], in1=o_sb[:, :, :], op=ALU.mult), [o_sb], [junk])
                        S.op("dve", lambda e: e.tensor_reduce(out=rst[:, 1, :], in_=jv, axis=AX.X, op=ALU.add), [junk], [rst])
                        S.op("dve", lambda e: e.tensor_scalar(out=rst[:, 2:4, :], in0=rst[:, 0:2, :], scalar1=1.0 / DV, scalar2=None, op0=ALU.mult), [rst], [rst])
                        S.op("dve", lambda e: e.tensor_tensor(out=rst[:, 4, :], in0=rst[:, 2, :], in1=rst[:, 2, :], op=ALU.mult), [rst], [rst])
                        S.op("dve", lambda e: e.tensor_tensor(out=rst[:, 5, :], in0=rst[:, 3, :], in1=rst[:, 4, :], op=ALU.subtract), [rst], [rst])
                        S.op("dve", lambda e: e.tensor_scalar(out=rst[:, 4, :], in0=rst[:, 5, :], scalar1=LN_EPS, scalar2=None, op0=ALU.add), [rst], [rst])
                        S.op("act", lambda e: e.sqrt(out=rst[:, 4, :], in_=rst[:, 4, :]), [rst], [rst])
                        S.op("dve", lambda e: e.reciprocal(out=rst[:, 4, :], in_=rst[:, 4, :]), [rst], [rst])
                        for h in range(RH):
                            S.op("dve", lambda e, h=h: e.tensor_scalar(out=o_sb[:, h, :], in0=o_sb[:, h, :], scalar1=rst[:, 2, h:h + 1],
                                                                       scalar2=rst[:, 4, h:h + 1], op0=ALU.subtract, op1=ALU.mult), [o_sb, rst], [o_sb])
                        of = o_sb[:, :, :].rearrange("p a b -> p (a b)")
                        S.op("dve", lambda e: e.tensor_tensor(out=of, in0=of, in1=normg[:, :], op=ALU.mult), [o_sb, normg], [o_sb])
                        yr = yret[0]
                        sr = srg[t % 2]
                        S.dma("sp", sr[:, :], srg_d[tok0 + t * 128: tok0 + (t + 1) * 128, :], reads=[srg_d], writes=[sr])
                        S.op("dve", lambda e, yr=yr, sr=sr: e.tensor_tensor(out=yr[:, :], in0=of, in1=sr[:, :], op=ALU.mult), [o_sb, sr], [yr])
                        S.dma("sp", yret_d[tok0 + t * 128: tok0 + (t + 1) * 128, :], yr[:, :], reads=[yr], writes=[yret_d])
                    S.barrier()
            S.barrier()

        with ExitStack() as esC:
            yaT = sb(esC, "yaT", [128, AQW // 128, T], BF16)
            yrT = sb(esC, "yrT", [128, RVW // 128, T], BF16)
            stage = [sb(esC, f"stageC{i}", [128, max(AQW, RVW)], BF16) for i in range(2)]
            wring = [sb(esC, f"wrC{i}", [128, KG, 512], BF16) for i in range(3)]
            gpan = [sb(esC, f"gpan{i}", [128, NT, 512], F32) for i in range(2)]
            m_sb = sb(esC, "m_sb", [128, NT, 512], F32)
            mb = [sb(esC, f"mb{i}", [128, 512], BF16) for i in range(2)]
            for st in range(NST):
                tok0 = st * T
                load_actT(esC, lambda t: yatt_d[tok0 + t * 128: tok0 + (t + 1) * 128, :], AQW, yaT, yatt_d, stage=stage, q="sp")
                load_actT(esC, lambda t: yret_d[tok0 + t * 128: tok0 + (t + 1) * 128, :], RVW, yrT, yret_d, stage=stage, q="sp")
                for c0 in range(0, D, 512):
                    S.dma("sp", gpan[0][:, :, :], sga_d[tok0:tok0 + T, c0:c0 + 512].rearrange("(t p) n -> p t n", p=128), reads=[sga_d], writes=[gpan[0]])
                    S.dma("sp", gpan[1][:, :, :], sgr_d[tok0:tok0 + T, c0:c0 + 512].rearrange("(t p) n -> p t n", p=128), reads=[sgr_d], writes=[gpan[1]])

                    def epi1(t, c0_, cn, ps):
                        S.op("dve", lambda e: e.tensor_tensor(out=m_sb[:, t, :], in0=ps[:, :], in1=gpan[0][:, t, :], op=ALU.mult), [ps, gpan[0]], [m_sb])

                    def epi2(t, c0_, cn, ps):
                        S.op("dve", lambda e: e.tensor_tensor(out=gpan[1][:, t, :], in0=ps[:, :], in1=gpan[1][:, t, :], op=ALU.mult), [ps, gpan[1]], [gpan[1]])
                        m_ = mb[t % 2]
                        S.op("dve", lambda e: e.tensor_tensor(out=m_[:, :], in0=gpan[1][:, t, :], in1=m_sb[:, t, :], op=ALU.add), [gpan[1], m_sb], [m_])
                        S.dma("sp", mrg_d[tok0 + t * 128: tok0 + (t + 1) * 128, c0_:c0_ + 512], m_[:, :], reads=[m_], writes=[mrg_d])
                    gemm(yaT, AQW // 128, w_ao, [(c0, 512, "B", 0)], wring, epi_B=epi1, psB=(0, 1, 2, 3))
                    gemm(yrT, RVW // 128, w_ro, [(c0, 512, "B", 0)], wring, epi_B=epi2, psB=(4, 5, 0, 1))
            S.barrier()

        T2, NT2 = 256, 2

        def resid_ln_phase(name, src_bf_d, K_src, w2d, resid_d, ln_i, out32_tb, outbf_tb, gate=None):
            with ExitStack() as esD:
                aT = sb(esD, name + "aT", [128, K_src // 128, T2], BF16)
                stage = [sb(esD, name + "stg0", [128, K_src], BF16)]
                wring = [sb(esD, f"{name}wr{i}", [128, KG, 512], BF16) for i in range(3)]
                z = sb(esD, name + "z", [128, NT2, D], F32)
                xpan = [sb(esD, f"{name}xp{i}", [128, NT2, 512], F32) for i in range(2)]
                g_bc = load_bc(esD, name + "g", ln_d[ln_i][0], D)
                b_bc = load_bc(esD, name + "b", ln_d[ln_i][1], D)
                zb = sb(esD, name + "zb", [128, D], BF16)
                if gate is not None:
                    pT = sb(esD, name + "pT", [128, PLE // 128, T2], BF16)
                    wpl = sb(esD, name + "wpl", [128, PLE // 128, 512], BF16)
                    sg = [sb(esD, f"{name}sg{i}", [128, 512], F32) for i in range(2)]
                xp_rr = [0]
                for st in range(TOK // T2):
                    tok0 = st * T2
                    load_actT(esD, lambda t: src_bf_d[tok0 + t * 128: tok0 + (t + 1) * 128, :], K_src, aT, src_bf_d, stage=stage, nt=NT2, q="sp")
                    if gate is not None:
                        load_actT(esD, lambda t: gate[0][tok0 + t * 128: tok0 + (t + 1) * 128, :], PLE, pT, None, stage=stage, nt=NT2, q="pool")
                    for c0 in range(0, D, 512):
                        xp = xpan[xp_rr[0] % 2]
                        xp_rr[0] += 1
                        S.dma("sp", xp[:, :, :], resid_d[tok0:tok0 + T2, c0:c0 + 512].rearrange("(t p) n -> p t n", p=128),
                              reads=[resid_d] if isinstance(resid_d, TB) else [], writes=[xp])
                        if gate is None:
                            def epi(t, c0_, cn, ps, xp=xp):
                                S.op("dve", lambda e: e.scalar_tensor_tensor(out=z[:, t, c0_:c0_ + 512], in0=xp[:, t, :], scalar=alpha, in1=ps[:, :],
                                                                             op0=ALU.mult, op1=ALU.add), [xp, ps], [z])
                        else:
                            S.dma("pool", wpl[:, :, :], wtile_kp(gate[1], 0, PLE // 128, c0, 512), writes=[wpl])

                            def epi(t, c0_, cn, ps, xp=xp):
                                s_ = sg[t % 2]
                                S.op("act", lambda e: e.activation(out=s_[:, :], in_=ps[:, :], func=AF.Sigmoid), [ps], [s_])
                                pp = PS[4 + t % 2]

                                def fn(e):
                                    for k in range(PLE // 128):
                                        inst = e.matmul(pp[:, :], pT[:, k, t * 128:(t + 1) * 128], wpl[:, k, :], start=(k == 0), stop=(k == PLE // 128 - 1))
                                    return inst
                                S.op("pe", fn, [pT, wpl], [pp])
                                S.op("dve", lambda e: e.tensor_tensor(out=s_[:, :], in0=s_[:, :], in1=pp[:, :], op=ALU.mult), [s_, pp], [s_])
                                S.op("dve", lambda e: e.scalar_tensor_tensor(out=z[:, t, c0_:c0_ + 512], in0=xp[:, t, :], scalar=alpha, in1=s_[:, :],
                                                                             op0=ALU.mult, op1=ALU.add), [xp, s_], [z])
                        gemm(aT, K_src // 128, w2d, [(c0, 512, "B", 0)], wring, epi_B=epi, tiles=range(NT2))
                    for t in range(NT2):
                        rows = slice(tok0 + t * 128, tok0 + (t + 1) * 128)
                        layer_norm(esD, z, z[:, t, :], g_bc, b_bc, zb, out32_tb[rows, :], out32_tb,
                                   outbf_tb[rows, :] if outbf_tb is not None else None, outbf_tb, zb)
                S.barrier()

        resid_ln_phase("D", mrg_d, D, w_out, x_d, 0, x1_d, x1b_d)

        NTT = TOK // 128
        NB = CAP // 128
        with ExitStack() as esE:
            gates = sb(esE, "gates", [128, NTT, NE], F32)
            maskb = sb(esE, "maskb", [128, NTT, NE], BF16)
            aux = sb(esE, "aux", [128, NTT, 2 + NE], F32)
            slotf = sb(esE, "slotf", [128, NTT, NE], F32)
            utri = sb(esE, "utri", [128, 128], BF16)
            onesq = sb(esE, "onesq", [128, 128], BF16)
            iota = sb(esE, "iota", [128, CAP], F32)
            dummy = sb(esE, "dummy", [128, NB], F32)
            S.dma("pool", utri[:, :], c_utri[:, :], writes=[utri])
            S.op("dve", lambda e: e.memset(onesq[:, :], 1.0), writes=[onesq])
            S.dma("sp", iota[:, :], c_iota[:, :], writes=[iota])
            S.dma("sp", dummy[:, :], c_dummy[:, :], writes=[dummy])
            S.dma("sp", slotf[:, :, 0], c_tokid[:, :], writes=[slotf]) if False else None
            tkid = sb(esE, "tkid", [128, NTT], F32)
            S.dma("sp", tkid[:, :], c_tokid[:, :], writes=[tkid])
            S.op("dve", lambda e: e.tensor_copy(out=aux[:, :, 0], in_=tkid[:, :]), [tkid], [aux])
            S.op("dve", lambda e: e.memset(aux[:, :, 1], 1.0), writes=[aux])
            with ExitStack() as esE0:
                hT = sb(esE0, "hT", [128, KC, T], BF16)
                stage = [sb(esE0, "stageE0", [128, D], BF16)]
                wrt = sb(esE0, "wrt", [128, KC, NE], BF16)
                brt = load_bc(esE0, "brt", b_rt, NE)
                bdn = sb(esE0, "bdn", [NE, D], BF16)
                gT = sb(esE0, "gT", [NE, 128], BF16)
                gbf = sb(esE0, "gbf", [128, NE], BF16)
                lg = sb(esE0, "lg", [128, NE], F32)
                m8 = sb(esE0, "m8", [128, 16], F32)
                xin = [sb(esE0, f"xin{i}", [128, D], F32) for i in range(2)]
                S.op("dve", lambda e: e.memset(stage[0][:, :], 0.0), writes=[stage[0]])
                for b_ in range(NB):
                    S.dma("sp", x1b_d[TOK + b_ * 128: TOK + (b_ + 1) * 128, :], stage[0][:, :], reads=[stage[0]], writes=[x1b_d])
                S.dma("pool", wrt[:, :, :], w_rt.rearrange("(kc p) n -> p kc n", p=128), writes=[wrt])
                S.dma("pool", bdn[:, :], b_dn[:, :], writes=[bdn])
                for st in range(NST):
                    tok0 = st * T
                    load_actT(esE0, lambda t: x1b_d[tok0 + t * 128: tok0 + (t + 1) * 128, :], D, hT, x1b_d, stage=stage, q="sp")
                    for t in range(NT):
                        ti = st * NT + t
                        rows = slice(tok0 + t * 128, tok0 + (t + 1) * 128)
                        ps = PS[6 + t % 2]

                        def fn(e, t=t, ps=ps):
                            for k in range(KC):
                                inst = e.matmul(ps[:, 0:NE], hT[:, k, t * 128:(t + 1) * 128], wrt[:, k, :], start=(k == 0), stop=(k == KC - 1))
                            return inst
                        S.op("pe", fn, [hT, wrt], [ps])
                        gt = gates[:, ti, :]
                        S.op("dve", lambda e, ps=ps: e.tensor_tensor(out=lg[:, :], in0=ps[:, 0:NE], in1=brt[:, :], op=ALU.add), [ps, brt], [lg])
                        S.op("dve", lambda e: e.max(out=m8[:, 0:8], in_=lg[:, :]), [lg], [m8])
                        S.op("dve", lambda e: e.tensor_scalar(out=m8[:, 8:9], in0=m8[:, 0:1], scalar1=-1.0, scalar2=None, op0=ALU.mult), [m8], [m8])
                        S.op("dve", lambda e, gt=gt: e.tensor_scalar(out=gt, in0=lg[:, :], scalar1=m8[:, TOPK - 1:TOPK], scalar2=None, op0=ALU.is_ge), [lg, m8], [gates])
                        S.op("act", lambda e, ti=ti, gt=gt: e.copy(out=maskb[:, ti, :], in_=gt), [gates], [maskb])
                        S.op("act", lambda e: e.activation(out=lg[:, :], in_=lg[:, :], func=AF.Exp, bias=m8[:, 8:9], scale=1.0), [lg, m8], [lg])
                        S.op("dve", lambda e, gt=gt: e.tensor_tensor(out=gt, in0=gt, in1=lg[:, :], op=ALU.mult), [gates, lg], [gates])
                        S.op("dve", lambda e, gt=gt: e.reduce_sum(out=m8[:, 9:10], in_=gt, axis=AX.X), [gates], [m8])
                        S.op("dve", lambda e: e.reciprocal(out=m8[:, 10:11], in_=m8[:, 9:10]), [m8], [m8])
                        S.op("dve", lambda e, gt=gt: e.tensor_scalar(out=gt, in0=gt, scalar1=m8[:, 10:11], scalar2=None, op0=ALU.mult), [gates, m8], [gates])
                        S.op("act", lambda e, ti=ti, gt=gt: e.copy(out=aux[:, ti, 2:2 + NE], in_=gt), [gates], [aux])
                        S.op("act", lambda e, gt=gt: e.copy(out=gbf[:, :], in_=gt), [gates], [gbf])
                        S.op("pe", lambda e, ps=ps: e.matmul(ps[0:NE, 128:256], gbf[:, :], ident[:, :], start=True, stop=True), [gbf, ident], [ps])
                        evac(gT[:, :], ps[0:NE, 128:256], [ps], [gT])
                        xi = xin[t % 2]
                        S.dma("sp", xi[:, :], x1_d[rows, :], reads=[x1_d], writes=[xi])
                        for c0 in range(0, D, 512):
                            pb = PS[4 + (c0 // 512) % 2]
                            S.op("pe", lambda e, pb=pb, c0=c0: e.matmul(pb[:, :], gT[:, :], bdn[:, c0:c0 + 512], start=True, stop=True), [gT, bdn], [pb])
                            S.op("dve", lambda e, pb=pb, c0=c0, xi=xi: e.scalar_tensor_tensor(out=xi[:, c0:c0 + 512], in0=xi[:, c0:c0 + 512], scalar=alpha, in1=pb[:, :],
                                                                                              op0=ALU.mult, op1=ALU.add), [xi, pb], [xi])
                        for c_, a_ in enumerate(acc_ds):
                            S.dma("sp", a_[rows, :], xi[:, c_ * RCH:(c_ + 1) * RCH], reads=[xi], writes=[a_])
                for ti in range(NTT):
                    ps = PS[ti % 2]

                    def fn(e, ti=ti, ps=ps):
                        inst = e.matmul(ps[:, 0:NE], utri[:, :], maskb[:, ti, :], start=True, stop=(ti == 0))
                        for tj in range(ti):
                            inst = e.matmul(ps[:, 0:NE], onesq[:, :], maskb[:, tj, :], start=False, stop=(tj == ti - 1))
                        return inst
                    S.op("pe", fn, [utri, onesq, maskb], [ps])
                    S.op("dve", lambda e, ti=ti, ps=ps: e.scalar_tensor_tensor(out=slotf[:, ti, :], in0=ps[:, 0:NE], scalar=1.0, in1=maskb[:, ti, :],
                                                                               op0=ALU.add, op1=ALU.mult), [ps, maskb], [slotf])
                    S.op("dve", lambda e, ti=ti: e.tensor_scalar(out=slotf[:, ti, :], in0=slotf[:, ti, :], scalar1=-1.0, scalar2=None, op0=ALU.add), [slotf], [slotf])
                S.barrier()
            with ExitStack() as esE1:
                aT = sb(esE1, "aTe", [128, KC, CAP], BF16)
                wgr = [sb(esE1, f"wg{i}", [128, KG, 2, PW], BF16) for i in range(2)]
                wdr = [sb(esE1, f"wd{i}", [128, FKC, 512], BF16) for i in range(2)]
                h1 = [sb(esE1, f"h1_{i}", [128, NB, PW], BF16) for i in range(2)]
                h1T = sb(esE1, "h1T", [128, FKC, CAP], BF16)
                bgu = [sb(esE1, "bgu0", [1, NPP, 2, PW], BF16)]
                sw = [sb(esE1, f"sw{i}", [128, 3, PW], F32) for i in range(2)]
                xg = [sb(esE1, f"xg{i}", [128, D], BF16) for i in range(2)]
                ysb = sb(esE1, "ysb", [128, NB, D], F32)
                Pm = [sb(esE1, f"Pm{i}", [128, CAP], F32) for i in range(2)]
                ie = sb(esE1, "ie", [128, NB, 2 + NE], F32)
                idf = sb(esE1, "idf", [128, 2, NB], F32)
                idx = [sb(esE1, f"idx{i}", [128, NB], mybir.dt.int32) for i in range(2)]
                ge = [sb(esE1, f"ge{i}", [128, NB], F32) for i in range(2)]
                kg = KG
                ngrp = KC // kg
                wg_rr = 0
                wd_rr = 0
                h1_rr = 0
                pm_rr = 0
                xg_rr = 0
                for ex in range(NE):
                    ix = idx[ex % 2]
                    gx = ge[ex % 2]
                    for ti in range(NTT):
                        pm = Pm[pm_rr % 2]
                        pm_rr += 1
                        S.op("dve", lambda e, pm=pm, ti=ti, ex=ex: e.tensor_scalar(out=pm[:, :], in0=iota[:, :], scalar1=slotf[:, ti, ex:ex + 1], scalar2=None, op0=ALU.is_equal),
                             [iota, slotf], [pm])
                        for b_ in range(NB):
                            pb = PS[b_]
                            S.op("pe", lambda e, pm=pm, ti=ti, b_=b_, pb=pb: e.matmul(pb[:, 0:2 + NE], pm[:, b_ * 128:(b_ + 1) * 128], aux[:, ti, :],
                                                                                      start=(ti == 0), stop=(ti == NTT - 1)), [pm, aux], [pb])
                            if ti == NTT - 1:
                                evac(ie[:, b_, :], pb[:, 0:2 + NE], [pb], [ie])
                    S.op("dve", lambda e: e.tensor_tensor(out=idf[:, 0, :], in0=ie[:, :, 1], in1=dummy[:, :], op=ALU.mult), [ie, dummy], [idf])
                    S.op("dve", lambda e: e.tensor_tensor(out=idf[:, 1, :], in0=ie[:, :, 0], in1=idf[:, 0, :], op=ALU.subtract), [ie, idf], [idf])
                    S.op("dve", lambda e: e.tensor_tensor(out=idf[:, 1, :], in0=idf[:, 1, :], in1=dummy[:, :], op=ALU.add), [idf, dummy], [idf])
                    S.op("dve", lambda e, ix=ix: e.tensor_copy(out=ix[:, :], in_=idf[:, 1, :]), [idf], [ix])
                    S.op("dve", lambda e, gx=gx, ex=ex: e.tensor_copy(out=gx[:, :], in_=ie[:, :, 2 + ex]), [ie], [gx])
                    for b_ in range(NB):
                        xg_ = xg[xg_rr % 2]
                        xg_rr += 1
                        S.idma(xg_[:, :], None, x1b_d[:, :], bass.IndirectOffsetOnAxis(ap=ix[:, b_:b_ + 1], axis=0),
                               reads=[x1b_d, ix], writes=[xg_])
                        for k0 in range(0, KC, 4):
                            kn = min(4, KC - k0)
                            pt = PS[6 + (k0 // 4) % 2]

                            def fnT(e, xg_=xg_, k0=k0, kn=kn, pt=pt):
                                for j in range(kn):
                                    inst = e.matmul(pt[:, j * 128:(j + 1) * 128], xg_[:, (k0 + j) * 128:(k0 + j + 1) * 128], ident[:, :], start=True, stop=True)
                                return inst
                            S.op("pe", fnT, [xg_, ident], [pt])
                            evac(aT[:, k0:k0 + kn, b_ * 128:(b_ + 1) * 128], pt[:, 0:kn * 128].rearrange("p (a b) -> p a b", b=128), [pt], [aT])
                    bg = bgu[0]
                    S.dma("pool", bg[0:1, :, 0, :], b_gu[ex:ex + 1, 0:FF].rearrange("o (a b) -> o a b", b=PW), writes=[bg])
                    S.dma("pool", bg[0:1, :, 1, :], b_gu[ex:ex + 1, FF:2 * FF].rearrange("o (a b) -> o a b", b=PW), writes=[bg])
                    wv = w_gu[ex].rearrange("(kc p) n -> p kc n", p=128)
                    for pp in range(NPP):
                        f0 = pp * PW
                        hp = h1[h1_rr % 2]
                        h1_rr += 1
                        for g in range(ngrp):
                            wt = wgr[wg_rr % 2]
                            wg_rr += 1
                            S.dma("pool", wt[:, 0:kg, 0, :], wv[:, g * kg:(g + 1) * kg, f0:f0 + PW], writes=[wt])
                            S.dma("pool", wt[:, 0:kg, 1, :], wv[:, g * kg:(g + 1) * kg, FF + f0:FF + f0 + PW], writes=[wt])
                            for t in range(NB):
                                ps = PS[t % 4] if NB <= 4 else PS[t % 6]
                                assert NB <= 6

                                def fn(e, g=g, wt=wt, t=t, ps=ps, pp=pp, bg=bg):
                                    if g == 0:
                                        e.matmul(ps[:, :], ones[0:1, :], bg[0:1, pp, :, :].rearrange("o a b -> o (a b)"), start=True, stop=False)
                                    for k in range(kg):
                                        inst = e.matmul(ps[:, :], aT[:, g * kg + k, t * 128:(t + 1) * 128], wt[:, k, :, :].rearrange("p a b -> p (a b)"),
                                                        start=False, stop=(g == ngrp - 1 and k == kg - 1))
                                    return inst
                                S.op("pe", fn, [aT, wt, bg, ones], [ps])
                                if g == ngrp - 1:
                                    s_ = sw[t % 2]
                                    S.op("dve", lambda e, ps=ps, s_=s_: e.tensor_scalar(out=s_[:, 0, :], in0=ps[:, 0:PW], scalar1=LIMIT, scalar2=None, op0=ALU.min), [ps], [s_])
                                    S.op("act", lambda e, s_=s_: e.activation(out=s_[:, 1, :], in_=s_[:, 0, :], func=AF.Sigmoid, scale=SW_ALPHA), [s_], [s_])
                                    S.op("dve", lambda e, ps=ps, s_=s_: e.tensor_scalar(out=s_[:, 2, :], in0=ps[:, PW:2 * PW], scalar1=-LIMIT, scalar2=LIMIT, op0=ALU.max, op1=ALU.min), [ps], [s_])
                                    S.op("dve", lambda e, s_=s_: e.scalar_tensor_tensor(out=s_[:, 2, :], in0=s_[:, 2, :], scalar=1.0, in1=s_[:, 0, :], op0=ALU.add, op1=ALU.mult), [s_], [s_])
                                    S.op("dve", lambda e, s_=s_, t=t, hp=hp, gx=gx: e.scalar_tensor_tensor(
                                        out=hp[:, t, :], in0=s_[:, 2, :], scalar=gx[:, t:t + 1], in1=s_[:, 1, :], op0=ALU.mult, op1=ALU.mult),
                                        [s_, gx], [hp])
                                    nkp = PW // 128
                                    pt = PS[6 + t % 2]

                                    def fnT2(e, hp=hp, t=t, pt=pt):
                                        for j in range(nkp):
                                            inst = e.matmul(pt[:, j * 128:(j + 1) * 128], hp[:, t, j * 128:(j + 1) * 128], ident[:, :], start=True, stop=True)
                                        return inst
                                    S.op("pe", fnT2, [hp, ident], [pt])
                                    evac(h1T[:, pp * nkp:(pp + 1) * nkp, t * 128:(t + 1) * 128], pt[:, 0:nkp * 128].rearrange("p (a b) -> p a b", b=128), [pt], [h1T])
                    wdv = w_dn[ex].rearrange("(kc p) n -> p kc n", p=128)
                    for c0 in range(0, D, 512):
                        wt = wdr[wd_rr % 2]
                        wd_rr += 1
                        S.dma("pool", wt[:, :, :], wdv[:, :, c0:c0 + 512], writes=[wt])
                        for t in range(NB):
                            ps = PS[4 + t % 2]

                            def fn(e, wt=wt, t=t, ps=ps):
                                for k in range(FKC):
                                    inst = e.matmul(ps[:, :], h1T[:, k, t * 128:(t + 1) * 128], wt[:, k, :], start=(k == 0), stop=(k == FKC - 1))
                                return inst
                            S.op("pe", fn, [h1T, wt], [ps])
                            evac(ysb[:, t, c0:c0 + 512], ps[:, :], [ps], [ysb])
                    for b_ in range(NB):
                        for c_, a_ in enumerate(acc_ds):
                            S.idma(a_[:, :], bass.IndirectOffsetOnAxis(ap=ix[:, b_:b_ + 1], axis=0), ysb[:, b_, c_ * RCH:(c_ + 1) * RCH], None,
                                   reads=[ysb, ix], writes=[a_], compute_op=ALU.add)
                S.barrier()
            with ExitStack() as esE2:
                g_bc = load_bc(esE2, "ln2g", ln_d[1][0], D)
                b_bc = load_bc(esE2, "ln2b", ln_d[1][1], D)
                zb = sb(esE2, "zb2", [128, D], BF16)
                zz = [sb(esE2, f"zz{i}", [128, D], F32) for i in range(2)]
                for ti in range(NTT):
                    rows = slice(ti * 128, (ti + 1) * 128)
                    z_ = zz[ti % 2]
                    for c_, a_ in enumerate(acc_ds):
                        S.dma("sp", z_[:, c_ * RCH:(c_ + 1) * RCH], a_[rows, :], reads=[a_], writes=[z_])
                    layer_norm(esE2, z_, z_[:, :], g_bc, b_bc, zb, x2_d[rows, :], x2_d, x2b_d[rows, :], x2b_d, zb)
                S.barrier()

        resid_ln_phase("F", x2b_d, D, w_pg, x2_d, 2, out_tb, None, gate=(p_d, w_ple))
        S.barrier()
    return nc


def host_consts(cfg, core):
    AH, RH = cfg["AH"], cfg["RH"]
    j = np.arange(128)[:, None]
    i = np.arange(128)[None, :]
    slopes = 2.0 ** (-8.0 * (np.arange(AH) + 1) / AH)
    tabA = np.zeros((128, 2, AH, 128), np.float64)
    for c in range(2):
        dist = (128 + i - j) if c == 0 else (i - j)
        valid = (dist >= 0) & (dist < 128)
        for h in range(AH):
            tabA[:, c, h, :] = np.where(valid, np.exp(-slopes[h] * dist), 0.0)
    nq = cfg["SEQ"] // cfg["TOK"]
    seq_start = (core % nq) == 0
    tabF = np.zeros((128, AH, 128)) if seq_start else tabA[:, 0].copy()
    gam = 1.0 - 2.0 ** (-5.0 - np.arange(RH))
    DT = np.zeros((128, RH, 128))
    qdec = np.zeros((128, RH, 128))
    kdec = np.zeros((128, RH))
    for h in range(RH):
        d = i - j
        DT[:, h, :] = np.where(d >= 0, gam[h] ** np.maximum(d, 0), 0.0) * DK ** -0.5
        qdec[:, h, :] = gam[h] ** (np.arange(128) + 1.0)[None, :]
        kdec[:, h] = gam[h] ** (127.0 - np.arange(128)) * DK ** -0.5
    f = lambda a: np.ascontiguousarray(a.reshape(128, -1).astype(np.float32))
    CAP, TOK = cfg["CAP"], cfg["TOK"]
    pidx = np.arange(128)
    extra = dict(
        c_utri=(pidx[:, None] < pidx[None, :]).astype(np.float32),
        c_iota=np.ascontiguousarray(np.broadcast_to(np.arange(CAP, dtype=np.float32)[None, :], (128, CAP))),
        c_dummy=(TOK + np.arange(CAP // 128)[None, :] * 128 + pidx[:, None]).astype(np.float32),
        c_tokid=(np.arange(TOK // 128)[None, :] * 128 + pidx[:, None]).astype(np.float32))
    return dict(**extra, c_ident=np.eye(128, dtype=np.float32), c_tabA=f(tabA), c_tabF=f(tabF), c_DT=f(DT), c_qdec=f(qdec), c_kdec=f(kdec))


def make_in_maps(cfg, inp):
    TOK, NPREV, SEQ, NC = cfg["TOK"], cfg["NPREV"], cfg["SEQ"], cfg["NCORE"]
    nq = SEQ // TOK
    x = np.asarray(inp["x"], np.float32)
    p = np.asarray(inp["p"], np.float32)[0]
    D = x.shape[-1]
    shared = {}
    for k in ("w_in", "w_att_out", "w_ret_out", "w_out", "w_router", "w_gate_up", "b_gate_up", "w_down", "b_down", "w_ple", "w_ple_gate"):
        shared[k] = np.ascontiguousarray(np.asarray(inp[k], np.float32)[0])
    for k in ("attn_sinks", "ret_norm_g", "ln1_g", "ln1_b", "ln2_g", "ln2_b", "ln3_g", "ln3_b", "b_router"):
        shared[k] = np.ascontiguousarray(np.asarray(inp[k], np.float32)[0][None, :])
    maps = []
    for c in range(NC):
        b, q = c // nq, c % nq
        m = dict(shared)
        m["x"] = np.ascontiguousarray(x[b, q * TOK:(q + 1) * TOK])
        xp = np.zeros((NPREV, D), np.float32)
        if q > 0:
            xp[NPREV - q * TOK:] = x[b, 0:q * TOK]
        m["xprev"] = xp
        m["p"] = np.ascontiguousarray(p[b, q * TOK:(q + 1) * TOK])
        m.update(host_consts(cfg, c))
        maps.append(m)
    return maps


def kernel(**inputs):
    cfg = FULL
    nc = build(cfg)
    maps = make_in_maps(cfg, inputs)
    res = run_bass_kernel_spmd(nc, maps, core_ids=list(range(cfg["NCORE"])))
    nq = cfg["SEQ"] // cfg["TOK"]
    out = np.zeros((cfg["BATCH"], cfg["SEQ"], cfg["D"]), np.float32)
    for c in range(cfg["NCORE"]):
        b, q = c // nq, c % nq
        out[b, q * cfg["TOK"]:(q + 1) * cfg["TOK"]] = res.results[c]["out"]
    return out
```

```python
import math
from contextlib import ExitStack
import numpy as np
import concourse.bass as bass
import concourse.mybir as mybir
from concourse.bass_utils import run_bass_kernel_spmd

F32 = mybir.dt.float32
BF16 = mybir.dt.bfloat16
ALU = mybir.AluOpType
AF = mybir.ActivationFunctionType
AX = mybir.AxisListType

FULL = dict(D=4096, AH=32, AKV=4, RH=8, NE=32, FF=1536, PLE=256, TOK=2048, NPREV=6144,
            SEQ=8192, BATCH=2, NCORE=8, CAP=384)
HD, DK, DV = 64, 128, 256
TOPK = 4
LN_EPS = 1e-5
LIMIT = 7.0
SW_ALPHA = 1.702
T = 512
NT = 4
PW = 256


class TB:
    def __init__(self, t, name):
        self.t = t
        self.name = name
        self.w = {}
        self.r = {}

    def __getitem__(self, k):
        return self.t[k]


class Sync:
    NDMA = 20

    def __init__(self, nc, es):
        self.nc = nc
        self.e = {}
        for nm, attr in (("pe", "tensor"), ("dve", "vector"), ("act", "scalar"), ("pool", "gpsimd"), ("sp", "sync")):
            sem = es.enter_context(nc.semaphore("s_" + nm))
            self.e[nm] = dict(eng=getattr(nc, attr), sem=sem, cnt=0, seen={})
        self.dq = {}
        for q in ("pool", "sp"):
            sems = [es.enter_context(nc.semaphore(f"d_{q}{i}")) for i in range(self.NDMA)]
            self.dq[q] = dict(sems=sems, val=[0] * self.NDMA, rr=0)

    def _wait(self, en, tok):
        sem, val, key = tok
        e = self.e[en]
        if e["seen"].get(key, 0) < val:
            e["eng"].wait_ge(sem, val)
            e["seen"][key] = val

    def _deps(self, en, reads, writes, skip_same):
        for b in reads:
            for key, tok in b.w.items():
                if skip_same and key == en:
                    continue
                self._wait(en, tok)
        for b in writes:
            for d in (b.w, b.r):
                for key, tok in d.items():
                    if skip_same and key == en:
                        continue
                    self._wait(en, tok)

    def _mark(self, tok, reads, writes):
        key = tok[2]
        for b in reads:
            b.r[key] = tok
        for b in writes:
            b.w[key] = tok

    def op(self, en, fn, reads=(), writes=()):
        e = self.e[en]
        self._deps(en, reads, writes, skip_same=(en == "pe"))
        inst = fn(e["eng"])
        e["cnt"] += 1
        inst.then_inc(e["sem"], 1)
        self._mark((e["sem"], e["cnt"], en), reads, writes)

    def dma(self, q, out, in_, reads=(), writes=(), **kw):
        self._deps(q, reads, writes, skip_same=False)
        d = self.dq[q]
        i = d["rr"]
        d["rr"] = (i + 1) % self.NDMA
        key = (q, i)
        if d["val"][i] > 0:
            self._wait(q, (d["sems"][i], d["val"][i], key))
        inst = self.e[q]["eng"].dma_start(out=out, in_=in_, **kw)
        d["val"][i] += 16
        inst.then_inc(d["sems"][i], 16)
        self._mark((d["sems"][i], d["val"][i], key), reads, writes)

    def idma(self, out, out_off, in_, in_off, reads=(), writes=(), **kw):
        q = "pool"
        self._deps(q, reads, writes, skip_same=False)
        d = self.dq[q]
        i = d["rr"]
        d["rr"] = (i + 1) % self.NDMA
        key = (q, i)
        if d["val"][i] > 0:
            self._wait(q, (d["sems"][i], d["val"][i], key))
        inst = self.e[q]["eng"].indirect_dma_start(out=out, out_offset=out_off, in_=in_, in_offset=in_off, **kw)
        d["val"][i] += 16
        inst.then_inc(d["sems"][i], 16)
        self._mark((d["sems"][i], d["val"][i], key), reads, writes)

    def barrier(self):
        toks = []
        for nm, e in self.e.items():
            if e["cnt"] > 0:
                toks.append((e["sem"], e["cnt"], nm))
        for q, d in self.dq.items():
            for i in range(self.NDMA):
                if d["val"][i] > 0:
                    toks.append((d["sems"][i], d["val"][i], (q, i)))
        for nm in self.e:
            for tok in toks:
                if tok[2] != nm:
                    self._wait(nm, tok)


def build(cfg):
    D, AH, AKV, RH, NE, FF, PLE = cfg["D"], cfg["AH"], cfg["AKV"], cfg["RH"], cfg["NE"], cfg["FF"], cfg["PLE"]
    TOK, NPREV = cfg["TOK"], cfg["NPREV"]
    G = AH // AKV
    KC = D // 128
    KG = min(16, KC)
    AQW, AKW, RQW, RVW = AH * HD, AKV * HD, RH * DK, RH * DV
    o_aq = 0
    o_ak = o_aq + AQW
    o_av = o_ak + AKW
    o_rq = o_av + AKW
    o_rk = o_rq + RQW
    o_rv = o_rk + RQW
    o_rg = o_rv + RVW
    o_ga = o_rg + RVW
    o_gr = o_ga + D
    INW = o_gr + D
    NST = TOK // T
    NPST = NPREV // T
    NPP = FF // PW
    FKC = FF // 128
    alpha = float((2 * 1) ** 0.25)
    gam = [1.0 - 2.0 ** (-5.0 - h) for h in range(RH)]
    g128 = [float(np.float32(g ** 128)) for g in gam]

    nc = bass.Bass("TRN2", target_bir_lowering=False)

    def din(name, shape):
        return nc.dram_tensor(name, list(shape), F32, kind="ExternalInput").ap()

    x_d = din("x", [TOK, D])
    xp_d = din("xprev", [NPREV, D])
    p_d = din("p", [TOK, PLE])
    w_in = din("w_in", [D, INW])
    sinks_d = din("attn_sinks", [1, AH])
    rng_d = din("ret_norm_g", [1, RVW])
    w_ao = din("w_att_out", [AQW, D])
    w_ro = din("w_ret_out", [RVW, D])
    w_out = din("w_out", [D, D])
    ln_d = [(din(f"ln{i}_g", [1, D]), din(f"ln{i}_b", [1, D])) for i in (1, 2, 3)]
    w_rt = din("w_router", [D, NE])
    b_rt = din("b_router", [1, NE])
    w_gu = din("w_gate_up", [NE, D, 2 * FF])
    b_gu = din("b_gate_up", [NE, 2 * FF])
    w_dn = din("w_down", [NE, FF, D])
    b_dn = din("b_down", [NE, D])
    w_ple = din("w_ple", [PLE, D])
    w_pg = din("w_ple_gate", [D, D])
    c_ident = din("c_ident", [128, 128])
    c_tabA = din("c_tabA", [128, 2 * AH * 128])
    c_tabF = din("c_tabF", [128, AH * 128])
    c_DT = din("c_DT", [128, RH * 128])
    c_qdec = din("c_qdec", [128, RH * 128])
    c_kdec = din("c_kdec", [128, RH])
    CAP = cfg["CAP"]
    c_utri = din("c_utri", [128, 128])
    c_iota = din("c_iota", [128, CAP])
    c_dummy = din("c_dummy", [128, CAP // 128])
    c_tokid = din("c_tokid", [128, 2 * (TOK // 128)])
    out_d = nc.dram_tensor("out", [TOK, D], F32, kind="ExternalOutput").ap()

    def scratch(name, shape, dt):
        return TB(nc.dram_tensor(name, list(shape), dt, kind="Internal").ap(), name)

    yatt_d = scratch("yatt_d", [TOK, AQW], BF16)
    yret_d = scratch("yret_d", [TOK, RVW], BF16)
    sga_d = scratch("sga_d", [TOK, D], F32)
    sgr_d = scratch("sgr_d", [TOK, D], F32)
    mrg_d = scratch("mrg_d", [TOK, D], BF16)
    srg_d = scratch("srg_d", [TOK, RVW], BF16)
    x1_d = scratch("x1_d", [TOK, D], F32)
    x1b_d = scratch("x1b_d", [TOK + cfg["CAP"], D], BF16)
    RCH = min(2048, D)
    acc_ds = [scratch(f"acc_d{c}", [TOK + cfg["CAP"], RCH], F32) for c in range(D // RCH)]
    x2_d = scratch("x2_d", [TOK, D], F32)
    x2b_d = scratch("x2b_d", [TOK, D], BF16)
    out_tb = TB(out_d, "out")

    with ExitStack() as es0:
        S = Sync(nc, es0)

        uniq = [0]

        def sb(es, name, shape, dt):
            uniq[0] += 1
            return TB(es.enter_context(nc.sbuf_tensor(f"{name}_{uniq[0]}", list(shape), dt)), name)

        PS = [TB(es0.enter_context(nc.psum_tensor(f"ps{i}", [128, 512], F32)), f"ps{i}") for i in range(8)]
        ident = sb(es0, "ident", [128, 128], BF16)
        ones = sb(es0, "ones", [1, 128], BF16)
        S.dma("pool", ident[:, :], c_ident[:, :], writes=[ident])
        S.op("dve", lambda e: e.memset(ones[:, :], 1.0), writes=[ones])
        evac_rr = [0]

        def evac(out_ap, in_ap, reads, writes):
            evac_rr[0] ^= 1
            if evac_rr[0]:
                S.op("act", lambda e: e.copy(out=out_ap, in_=in_ap), reads, writes)
            else:
                S.op("dve", lambda e: e.tensor_copy(out=out_ap, in_=in_ap), reads, writes)

        def load_actT(es_stage, src_ap_fn, K, actT, src_tb, tps=(6, 7), stage=None, nt=NT, q="pool"):
            kc_n = K // 128
            for t in range(nt):
                stg = stage[t % len(stage)]
                S.dma(q, stg[:, 0:K], src_ap_fn(t), reads=[src_tb] if src_tb else [], writes=[stg])
                for k0 in range(0, kc_n, 4):
                    kn = min(4, kc_n - k0)
                    ps = PS[tps[(k0 // 4) % 2]]

                    def fn(e, k0=k0, kn=kn, ps=ps, stg=stg):
                        for j in range(kn):
                            inst = e.matmul(ps[:, j * 128:(j + 1) * 128], stg[:, (k0 + j) * 128:(k0 + j + 1) * 128],
                                            ident[:, :], start=True, stop=True)
                        return inst
                    S.op("pe", fn, reads=[stg, ident], writes=[ps])
                    evac(actT[:, k0:k0 + kn, t * 128:(t + 1) * 128],
                         ps[:, 0:kn * 128].rearrange("p (a b) -> p a b", b=128), [ps], [actT])

        def wtile_kp(w_ap2d, k0, nk, c0, cn):
            return w_ap2d.rearrange("(kc p) n -> p kc n", p=128)[:, k0:k0 + nk, c0:c0 + cn]

        def gemm(actT, kc_n, w_ap2d, panels, wring, epi_B=None, epi_A=None, bias_fn=None, tiles=range(NT), psB=(0, 1, 2, 3), psA=(4, 5)):
            kg = min(KG, kc_n)
            ngrp = kc_n // kg
            for (c0, cn, kind, M) in panels:
                wts = []
                for g in range(ngrp):
                    wt = wring[wring_rr[0] % len(wring)]
                    wring_rr[0] += 1
                    S.dma("pool", wt[:, 0:kg, 0:cn], wtile_kp(w_ap2d, g * kg, kg, c0, cn), writes=[wt])
                    wts.append(wt)
                if kind == "B":
                    for g in range(ngrp):
                        wt = wts[g]
                        for t in tiles:
                            ps = PS[psB[t % len(psB)]]

                            def fn(e, g=g, wt=wt, t=t, ps=ps):
                                for k in range(kg):
                                    inst = e.matmul(ps[:, 0:cn], actT[:, g * kg + k, t * 128:(t + 1) * 128], wt[:, k, 0:cn],
                                                    start=(g == 0 and k == 0), stop=(g == ngrp - 1 and k == kg - 1))
                                return inst
                            S.op("pe", fn, reads=[actT, wt], writes=[ps])
                            if g == ngrp - 1:
                                epi_B(t, c0, cn, ps)
                else:
                    nblk = cn // M
                    for blk in range(nblk):
                        ps = PS[psA[blk % len(psA)]]

                        def fn(e, blk=blk, ps=ps):
                            for g in range(ngrp):
                                for k in range(kg):
                                    inst = e.matmul(ps[0:M, 0:T], wts[g][:, k, blk * M:(blk + 1) * M], actT[:, g * kg + k, 0:T],
                                                    start=(g == 0 and k == 0), stop=(g == ngrp - 1 and k == kg - 1))
                            return inst
                        S.op("pe", fn, reads=[actT] + wts, writes=[ps])
                        epi_A(c0 + blk * M, M, ps)

        wring_rr = [0]

        def layer_norm(es, z, zt_ap, g_bc, b_bc, junk, out32_ap, out32_tb, outbf_ap=None, outbf_tb=None, zb=None):
            st = ln_small
            S.op("dve", lambda e: e.reduce_sum(out=st[:, 0:1], in_=zt_ap, axis=AX.X), [z], [st])
            S.op("dve", lambda e: e.tensor_tensor(out=junk[:, 0:D], in0=zt_ap, in1=zt_ap, op=ALU.mult), [z], [junk])
            S.op("dve", lambda e: e.reduce_sum(out=st[:, 1:2], in_=junk[:, 0:D], axis=AX.X), [junk], [st])
            S.op("dve", lambda e: e.tensor_scalar(out=st[:, 2:4], in0=st[:, 0:2], scalar1=1.0 / D, scalar2=None, op0=ALU.mult), [st], [st])
            S.op("dve", lambda e: e.tensor_tensor(out=st[:, 4:5], in0=st[:, 2:3], in1=st[:, 2:3], op=ALU.mult), [st], [st])
            S.op("dve", lambda e: e.tensor_tensor(out=st[:, 5:6], in0=st[:, 3:4], in1=st[:, 4:5], op=ALU.subtract), [st], [st])
            S.op("dve", lambda e: e.tensor_scalar(out=st[:, 6:7], in0=st[:, 5:6], scalar1=LN_EPS, scalar2=None, op0=ALU.add), [st], [st])
            S.op("act", lambda e: e.sqrt(out=st[:, 6:7], in_=st[:, 6:7]), [st], [st])
            S.op("dve", lambda e: e.reciprocal(out=st[:, 6:7], in_=st[:, 6:7]), [st], [st])
            S.op("dve", lambda e: e.tensor_scalar(out=zt_ap, in0=zt_ap, scalar1=st[:, 2:3], scalar2=st[:, 6:7], op0=ALU.subtract, op1=ALU.mult), [z, st], [z])
            S.op("dve", lambda e: e.tensor_tensor(out=zt_ap, in0=zt_ap, in1=g_bc[:, :], op=ALU.mult), [z, g_bc], [z])
            S.op("dve", lambda e: e.tensor_tensor(out=zt_ap, in0=zt_ap, in1=b_bc[:, :], op=ALU.add), [z, b_bc], [z])
            S.dma("sp", out32_ap, zt_ap, reads=[z], writes=[out32_tb])
            if outbf_ap is not None:
                S.op("act", lambda e: e.copy(out=zb[:, 0:D], in_=zt_ap), [z], [zb])
                S.dma("sp", outbf_ap, zb[:, 0:D], reads=[zb], writes=[outbf_tb])

        ln_small = sb(es0, "ln_small", [128, 8], F32)

        def load_bc(es, name, src_ap, n):
            t_ = sb(es, name, [128, n], F32)
            S.dma("sp", t_[:, :], src_ap.partition_broadcast(128), writes=[t_])
            return t_

        with ExitStack() as esAB:
            xT = sb(esAB, "xT", [128, KC, T], BF16)
            stage = [sb(esAB, "stage0", [128, D], BF16)]
            wring = [sb(esAB, f"wr{i}", [128, KG, 512], BF16) for i in range(max(3, KC // KG))]
            kT = sb(esAB, "kT", [64, AKV, 128 + T], BF16)
            v1 = sb(esAB, "v1", [128, NT + 1, AKV, HD + 1], BF16)
            state = sb(esAB, "state", [128, RH, DV], F32)
            state_bf = sb(esAB, "state_bf", [128, RH, DV], BF16)
            kdec = sb(esAB, "kdec", [128, RH], F32)
            S.dma("sp", kdec[:, :], c_kdec[:, :], writes=[kdec])
            S.op("dve", lambda e: e.memset(v1[:, :, :, :], 1.0), writes=[v1])
            S.op("dve", lambda e: e.memset(state[:, :, :], 0.0), writes=[state])
            S.op("dve", lambda e: e.memset(state_bf[:, :, :], 0.0), writes=[state_bf])

            def kv_halo_epis():
                def epi_A(c, M, ps):
                    kvh = (c - o_ak) // HD
                    evac(kT[:, kvh, 0:128], ps[0:HD, T - 128:T], [ps], [kT])

                def epi_B(t, c0, cn, ps):
                    if t == NT - 1:
                        evac(v1[:, 0, :, 0:HD], ps[:, 0:AKW].rearrange("p (a b) -> p a b", b=HD), [ps], [v1])
                return epi_A, epi_B

            def ret_state_update(ksc, vsb, t, ps_banks=(6, 7)):
                for h0 in range(0, RH, 2):
                    ps = PS[ps_banks[(h0 // 2) % 2]]

                    def fn(e, h0=h0, ps=ps):
                        for j in range(2):
                            h = h0 + j
                            inst = e.matmul(ps[:, j * DV:(j + 1) * DV], ksc[:, t, h * DK:(h + 1) * DK], vsb[:, t, h * DV:(h + 1) * DV],
                                            start=True, stop=True)
                        return inst
                    S.op("pe", fn, reads=[ksc, vsb], writes=[ps])
                    for j in range(2):
                        h = h0 + j
                        S.op("dve", lambda e, h=h, j=j, ps=ps: e.scalar_tensor_tensor(
                            out=state[:, h, :], in0=state[:, h, :], scalar=g128[h], in1=ps[:, j * DV:(j + 1) * DV],
                            op0=ALU.mult, op1=ALU.add), [state, ps], [state])
                S.op("act", lambda e: e.copy(out=state_bf[:, :, :], in_=state[:, :, :]), [state], [state_bf])

            with ExitStack() as esA:
                ksc = sb(esA, "ksc", [128, NT, RQW], BF16)
                vsb = sb(esA, "vsb", [128, NT, RVW], BF16)
                for pst in range(NPST):
                    load_actT(esA, lambda t, pst=pst: xp_d[pst * T + t * 128: pst * T + (t + 1) * 128, :], D, xT, None, stage=stage)

                    def epi_B(t, c0, cn, ps):
                        if c0 < o_rv:
                            for hh in range(cn // DK):
                                h = (c0 - o_rk) // DK + hh
                                S.op("dve", lambda e, h=h, hh=hh: e.tensor_scalar(
                                    out=ksc[:, t, h * DK:(h + 1) * DK], in0=ps[:, hh * DK:(hh + 1) * DK], scalar1=kdec[:, h:h + 1],
                                    scalar2=None, op0=ALU.mult), [ps, kdec], [ksc])
                        else:
                            evac(vsb[:, t, c0 - o_rv:c0 - o_rv + cn], ps[:, 0:cn], [ps], [vsb])
                    panels = [(c, min(512, o_rg - c), "B", 0) for c in range(o_rk, o_rg, 512)]
                    panels = []
                    for c in range(o_rk, o_rv, 512):
                        panels.append((c, min(512, o_rv - c), "B", 0))
                    for c in range(o_rv, o_rg, 512):
                        panels.append((c, min(512, o_rg - c), "B", 0))
                    gemm(xT, KC, w_in, panels, wring, epi_B=epi_B)
                    if pst == NPST - 1:
                        eA, eB = kv_halo_epis()
                        gemm(xT, KC, w_in, [(o_ak, AKW, "A", HD)], wring, epi_A=eA)
                        gemm(xT, KC, w_in, [(o_av, AKW, "B", 0)], wring, epi_B=eB, tiles=[NT - 1])
                    for t in range(NT):
                        ret_state_update(ksc, vsb, t)
                S.barrier()

            for st in range(NST):
                tok0 = st * T
                load_actT(esAB, lambda t: x_d[tok0 + t * 128: tok0 + (t + 1) * 128, :], D, xT, None, stage=stage)
                with ExitStack() as esI:
                    qT = sb(esI, "qT", [64, AH, T], BF16)
                    tabA = sb(esI, "tabA", [128, 2, AH, 128], BF16)
                    tabF = sb(esI, "tabF", [128, AH, 128], BF16)
                    esink = sb(esI, "esink", [128, AH], F32)
                    e_sb = [sb(esI, f"e_sb{i}", [128, 512], BF16) for i in range(2)]
                    PT = [sb(esI, f"PT{i}", [128, 4, 128], BF16) for i in range(2)]
                    den = sb(esI, "den", [128, 8], F32)
                    yatt = [sb(esI, f"yatt{i}", [128, AQW], BF16) for i in range(2)]
                    S.dma("pool", tabA[:, :, :, :], c_tabA.rearrange("p (c h i) -> p c h i", c=2, h=AH), writes=[tabA])
                    S.dma("pool", tabF[:, :, :], c_tabF.rearrange("p (h i) -> p h i", h=AH), writes=[tabF])
                    S.dma("sp", esink[:, :], sinks_d.partition_broadcast(128), writes=[esink])
                    S.op("act", lambda e: e.activation(out=esink[:, :], in_=esink[:, :], func=AF.Exp), [esink], [esink])

                    def epi_Aq(c, M, ps):
                        if c < o_ak:
                            evac(qT[:, c // HD, :], ps[0:HD, 0:T], [ps], [qT])
                        else:
                            kvh = (c - o_ak) // HD
                            evac(kT[:, kvh, 128:128 + T], ps[0:HD, 0:T], [ps], [kT])

                    def epi_Bv(t, c0, cn, ps):
                        evac(v1[:, t + 1, :, 0:HD], ps[:, 0:AKW].rearrange("p (a b) -> p a b", b=HD), [ps], [v1])
                    panels = [(c, min(512, AQW - c), "A", HD) for c in range(0, AQW, 512)] + [(o_ak, AKW, "A", HD)]
                    gemm(xT, KC, w_in, panels, wring, epi_A=epi_Aq)
                    gemm(xT, KC, w_in, [(o_av, AKW, "B", 0)], wring, epi_B=epi_Bv)
                    for t in range(NT):
                        ya = yatt[t % 2]
                        for kvh in range(AKV):
                            for h0 in range(kvh * G, (kvh + 1) * G, 4):
                                nh = min(4, G)
                                for c in range(2):
                                    ps = PS[c]
                                    S.op("pe", lambda e, c=c, ps=ps, h0=h0, kvh=kvh, t=t: e.matmul(
                                        ps[:, 0:nh * 128], kT[:, kvh, (t + c) * 128:(t + c + 1) * 128],
                                        qT[:, h0:h0 + nh, t * 128:(t + 1) * 128], start=True, stop=True), [kT, qT], [ps])
                                    S.op("act", lambda e, c=c, ps=ps: e.activation(out=e_sb[c][:, 0:nh * 128], in_=ps[:, 0:nh * 128],
                                                                                   func=AF.Exp, scale=HD ** -0.5), [ps], [e_sb[c]])
                                    tab = tabF[:, h0:h0 + nh, :] if (st == 0 and t == 0 and c == 0) else tabA[:, c, h0:h0 + nh, :]
                                    S.op("dve", lambda e, c=c, tab=tab: e.tensor_tensor(
                                        out=PT[c][:, 0:nh, :], in0=e_sb[c][:, 0:nh * 128].rearrange("p (a b) -> p a b", b=128), in1=tab,
                                        op=ALU.mult), [e_sb[c], tabA, tabF], [PT[c]])
                                pso = PS[2 + ((h0 // 4) % 2)]

                                def fn(e, pso=pso, kvh=kvh, t=t):
                                    for hh in range(nh):
                                        e.matmul(pso[:, hh * 128:hh * 128 + HD + 1], PT[0][:, hh, :], v1[:, t, kvh, :], start=True, stop=False)
                                        inst = e.matmul(pso[:, hh * 128:hh * 128 + HD + 1], PT[1][:, hh, :], v1[:, t + 1, kvh, :], start=False, stop=True)
                                    return inst
                                S.op("pe", fn, reads=[PT[0], PT[1], v1], writes=[pso])
                                pv = pso[:, 0:nh * 128].rearrange("p (a b) -> p a b", b=128)
                                S.op("dve", lambda e, pv=pv, h0=h0: e.tensor_tensor(out=den[:, 0:nh], in0=pv[:, :, HD], in1=esink[:, h0:h0 + nh], op=ALU.add),
                                     [pso, esink], [den])
                                S.op("dve", lambda e: e.reciprocal(out=den[:, 4:4 + nh], in_=den[:, 0:nh]), [den], [den])
                                for hh in range(nh):
                                    h = h0 + hh
                                    S.op("act", lambda e, hh=hh, h=h, pso=pso, ya=ya: e.activation(
                                        out=ya[:, h * HD:(h + 1) * HD], in_=pso[:, hh * 128:hh * 128 + HD], func=AF.Copy,
                                        scale=den[:, 4 + hh:5 + hh]), [pso, den], [ya])
                        S.dma("sp", yatt_d[tok0 + t * 128: tok0 + (t + 1) * 128, :], ya[:, :], reads=[ya], writes=[yatt_d])
                    S.op("act", lambda e: e.copy(out=kT[:, :, 0:128], in_=kT[:, :, T:T + 128]), [kT], [kT])
                    S.op("dve", lambda e: e.tensor_copy(out=v1[:, 0, :, :], in_=v1[:, NT, :, :]), [v1], [v1])
                    S.barrier()
                with ExitStack() as esII:
                    rqT = sb(esII, "rqT", [128, RH, T], BF16)
                    rkT = sb(esII, "rkT", [128, RH, T], BF16)
                    ksc = sb(esII, "ksc2", [128, NT, RQW], BF16)
                    vsb = sb(esII, "vsb2", [128, NT, RVW], BF16)
                    srg = [sb(esII, f"srg{i}", [128, RVW], BF16) for i in range(2)]
                    gsb = [sb(esII, f"gsb{i}", [128, 512], BF16) for i in range(2)]
                    DT = sb(esII, "DT", [128, RH, 128], F32)
                    qdec = sb(esII, "qdec", [128, RH, 128], BF16)
                    normg = load_bc(esII, "normg", rng_d, RVW)
                    AT = sb(esII, "AT", [128, RH, 128], BF16)
                    rqs = sb(esII, "rqs", [128, RH, 128], BF16)
                    o_sb = sb(esII, "o_sb", [128, RH, DV], F32)
                    junk = stage[0]
                    rst = sb(esII, "rst", [128, 6, RH], F32)
                    yret = [sb(esII, "yret0", [128, RVW], BF16)]
                    gst = [sb(esII, f"gst{i}", [128, 512], F32) for i in range(2)]
                    S.dma("sp", DT[:, :, :], c_DT.rearrange("p (h i) -> p h i", h=RH), writes=[DT])
                    S.dma("pool", qdec[:, :, :], c_qdec.rearrange("p (h i) -> p h i", h=RH), writes=[qdec])

                    def epi_Ar(c, M, ps):
                        if c < o_rk:
                            evac(rqT[:, (c - o_rq) // DK, :], ps[:, 0:T], [ps], [rqT])
                        else:
                            evac(rkT[:, (c - o_rk) // DK, :], ps[:, 0:T], [ps], [rkT])
                    gst_rr = [0]

                    def epi_Br(t, c0, cn, ps):
                        if c0 < o_rv:
                            for hh in range(cn // DK):
                                h = (c0 - o_rk) // DK + hh
                                S.op("dve", lambda e, h=h, hh=hh: e.tensor_scalar(
                                    out=ksc[:, t, h * DK:(h + 1) * DK], in0=ps[:, hh * DK:(hh + 1) * DK], scalar1=kdec[:, h:h + 1],
                                    scalar2=None, op0=ALU.mult), [ps, kdec], [ksc])
                        elif c0 < o_rg:
                            evac(vsb[:, t, c0 - o_rv:c0 - o_rv + cn], ps[:, 0:cn], [ps], [vsb])
                        elif c0 < o_ga:
                            g_ = gsb[gst_rr[0] % 2]
                            gst_rr[0] += 1
                            S.op("act", lambda e: e.activation(out=g_[:, 0:cn], in_=ps[:, 0:cn], func=AF.Silu), [ps], [g_])
                            S.dma("sp", srg_d[tok0 + t * 128: tok0 + (t + 1) * 128, c0 - o_rg:c0 - o_rg + cn], g_[:, 0:cn], reads=[g_], writes=[srg_d])
                        else:
                            g_ = gst[gst_rr[0] % 2]
                            gst_rr[0] += 1
                            S.op("act", lambda e: e.activation(out=g_[:, 0:cn], in_=ps[:, 0:cn], func=AF.Sigmoid), [ps], [g_])
                            dst, cc = (sga_d, c0 - o_ga) if c0 < o_gr else (sgr_d, c0 - o_gr)
                            S.dma("sp", dst[tok0 + t * 128: tok0 + (t + 1) * 128, cc:cc + cn], g_[:, 0:cn], reads=[g_], writes=[dst])
                    panels = [(c, min(512, o_rv - c), "A", DK) for c in range(o_rq, o_rv, 512)]
                    gemm(xT, KC, w_in, panels, wring, epi_A=epi_Ar)
                    panels = []
                    for lo, hi in ((o_rk, o_rv), (o_rv, o_rg), (o_rg, o_ga), (o_ga, o_gr), (o_gr, INW)):
                        for c in range(lo, hi, 512):
                            panels.append((c, min(512, hi - c), "B", 0))
                    gemm(xT, KC, w_in, panels, wring, epi_B=epi_Br)
                    for t in range(NT):
                        tsl = slice(t * 128, (t + 1) * 128)
                        for hg in range(0, RH, 4):
                            nh = min(4, RH - hg)
                            ps = PS[(hg // 4) % 2]

                            def fn(e, hg=hg, nh=nh, ps=ps):
                                for j in range(nh):
                                    inst = e.matmul(ps[:, j * 128:(j + 1) * 128], rkT[:, hg + j, tsl], rqT[:, hg + j, tsl], start=True, stop=True)
                                return inst
                            S.op("pe", fn, [rkT, rqT], [ps])
                            S.op("dve", lambda e, hg=hg, nh=nh, ps=ps: e.tensor_tensor(
                                out=AT[:, hg:hg + nh, :], in0=ps[:, 0:nh * 128].rearrange("p (a b) -> p a b", b=128), in1=DT[:, hg:hg + nh, :],
                                op=ALU.mult), [ps, DT], [AT])
                        S.op("dve", lambda e: e.tensor_tensor(out=rqs[:, :, :], in0=rqT[:, :, tsl], in1=qdec[:, :, :], op=ALU.mult), [rqT, qdec], [rqs])
                        for h0 in range(0, RH, 2):
                            ps = PS[2 + (h0 // 2) % 4]

                            def fn(e, h0=h0, ps=ps):
                                for j in range(2):
                                    h = h0 + j
                                    e.matmul(ps[:, j * DV:(j + 1) * DV], AT[:, h, :], vsb[:, t, h * DV:(h + 1) * DV], start=True, stop=False)
                                    inst = e.matmul(ps[:, j * DV:(j + 1) * DV], rqs[:, h, :], state_bf[:, h, :], start=False, stop=True)
                                return inst
                            S.op("pe", fn, [AT, vsb, rqs, state_bf], [ps])
                            evac(o_sb[:, h0:h0 + 2, :], ps[:, 0:2 * DV].rearrange("p (a b) -> p a b", b=DV), [ps], [o_sb])
                        ret_state_update(ksc, vsb, t)
                        S.op("dve", lambda e: e.tensor_reduce(out=rst[:, 0, :], in_=o_sb[:, :, :], axis=AX.X, op=ALU.add), [o_sb], [rst])
                        jv = junk[:, 0:RVW].rearrange("p (a b) -> p a b", b=DV)
                        S.op("dve", lambda e: e.tensor_tensor(out=jv, in0=o_sb[:, :, :], in1=o_sb[:, :, :], op=ALU.mult), [o_sb], [junk])
                        S.op("dve", lambda e: e.tensor_reduce(out=rst[:, 1, :], in_=jv, axis=AX.X, op=ALU.add), [junk], [rst])
                        S.op("dve", lambda e: e.tensor_scalar(out=rst[:, 2:4, :], in0=rst[:, 0:2, :], scalar1=1.0 / DV, scalar2=None, op0=ALU.mult), [rst], [rst])
                        S.op("dve", lambda e: e.tensor_tensor(out=rst[:, 4, :], in0=rst[:, 2, :], in1=rst[:, 2, :], op=ALU.mult), [rst], [rst])
                        S.op("dve", lambda e: e.tensor_tensor(out=rst[:, 5, :], in0=rst[:, 3, :], in1=rst[:, 4, :], op=ALU.subtract), [rst], [rst])
                        S.op("dve", lambda e: e.tensor_scalar(out=rst[:, 4, :], in0=rst[:, 5, :], scalar1=LN_EPS, scalar2=None, op0=ALU.add), [rst], [rst])
                        S.op("act", lambda e: e.sqrt(out=rst[:, 4, :], in_=rst[:, 4, :]), [rst], [rst])
                        S.op("dve", lambda e: e.reciprocal(out=rst[:, 4, :], in_=rst[:, 4, :]), [rst], [rst])
                        for h in range(RH):
                            S.op("dve", lambda e, h=h: e.tensor_scalar(out=o_sb[:, h, :], in0=o_sb[:, h, :], scalar1=rst[:, 2, h:h + 1],
                                                                       scalar2=rst[:, 4, h:h + 1], op0=ALU.subtract, op1=ALU.mult), [o_sb, rst], [o_sb])
                        of = o_sb[:, :, :].rearrange("p a b -> p (a b)")
                        S.op("dve", lambda e: e.tensor_tensor(out=of, in0=of, in1=normg[:, :], op=ALU.mult), [o_sb, normg], [o_sb])
                        yr = yret[0]
                        sr = srg[t % 2]
                        S.dma("sp", sr[:, :], srg_d[tok0 + t * 128: tok0 + (t + 1) * 128, :], reads=[srg_d], writes=[sr])
                        S.op("dve", lambda e, yr=yr, sr=sr: e.tensor_tensor(out=yr[:, :], in0=of, in1=sr[:, :], op=ALU.mult), [o_sb, sr], [yr])
                        S.dma("sp", yret_d[tok0 + t * 128: tok0 + (t + 1) * 128, :], yr[:, :], reads=[yr], writes=[yret_d])
                    S.barrier()
            S.barrier()

        with ExitStack() as esC:
            yaT = sb(esC, "yaT", [128, AQW // 128, T], BF16)
            yrT = sb(esC, "yrT", [128, RVW // 128, T], BF16)
            stage = [sb(esC, f"stageC{i}", [128, max(AQW, RVW)], BF16) for i in range(2)]
            wring = [sb(esC, f"wrC{i}", [128, KG, 512], BF16) for i in range(3)]
            gpan = [sb(esC, f"gpan{i}", [128, NT, 512], F32) for i in range(2)]
            m_sb = sb(esC, "m_sb", [128, NT, 512], F32)
            mb = [sb(esC, f"mb{i}", [128, 512], BF16) for i in range(2)]
            for st in range(NST):
                tok0 = st * T
                load_actT(esC, lambda t: yatt_d[tok0 + t * 128: tok0 + (t + 1) * 128, :], AQW, yaT, yatt_d, stage=stage, q="sp")
                load_actT(esC, lambda t: yret_d[tok0 + t * 128: tok0 + (t + 1) * 128, :], RVW, yrT, yret_d, stage=stage, q="sp")
                for c0 in range(0, D, 512):
                    S.dma("sp", gpan[0][:, :, :], sga_d[tok0:tok0 + T, c0:c0 + 512].rearrange("(t p) n -> p t n", p=128), reads=[sga_d], writes=[gpan[0]])
                    S.dma("sp", gpan[1][:, :, :], sgr_d[tok0:tok0 + T, c0:c0 + 512].rearrange("(t p) n -> p t n", p=128), reads=[sgr_d], writes=[gpan[1]])

                    def epi1(t, c0_, cn, ps):
                        S.op("dve", lambda e: e.tensor_tensor(out=m_sb[:, t, :], in0=ps[:, :], in1=gpan[0][:, t, :], op=ALU.mult), [ps, gpan[0]], [m_sb])

                    def epi2(t, c0_, cn, ps):
                        S.op("dve", lambda e: e.tensor_tensor(out=gpan[1][:, t, :], in0=ps[:, :], in1=gpan[1][:, t, :], op=ALU.mult), [ps, gpan[1]], [gpan[1]])
                        m_ = mb[t % 2]
                        S.op("dve", lambda e: e.tensor_tensor(out=m_[:, :], in0=gpan[1][:, t, :], in1=m_sb[:, t, :], op=ALU.add), [gpan[1], m_sb], [m_])
                        S.dma("sp", mrg_d[tok0 + t * 128: tok0 + (t + 1) * 128, c0_:c0_ + 512], m_[:, :], reads=[m_], writes=[mrg_d])
                    gemm(yaT, AQW // 128, w_ao, [(c0, 512, "B", 0)], wring, epi_B=epi1, psB=(0, 1, 2, 3))
                    gemm(yrT, RVW // 128, w_ro, [(c0, 512, "B", 0)], wring, epi_B=epi2, psB=(4, 5, 0, 1))
            S.barrier()

        T2, NT2 = 512, 4

        def resid_ln_phase(name, src_bf_d, K_src, w2d, resid_d, ln_i, out32_tb, outbf_tb, gate=None):
            with ExitStack() as esD:
                aT = sb(esD, name + "aT", [128, K_src // 128, T2], BF16)
                stage = [sb(esD, name + "stg0", [128, K_src], BF16)]
                wring = [sb(esD, f"{name}wr{i}", [128, KG, 512], BF16) for i in range(2)]
                z = sb(esD, name + "z", [128, NT2, D], F32)
                xpan = [sb(esD, f"{name}xp{i}", [128, NT2, 512], F32) for i in range(2)]
                g_bc = load_bc(esD, name + "g", ln_d[ln_i][0], D)
                b_bc = load_bc(esD, name + "b", ln_d[ln_i][1], D)
                zb = sb(esD, name + "zb", [128, D], BF16)
                if gate is not None:
                    pT = sb(esD, name + "pT", [128, PLE // 128, T2], BF16)
                    wpl = sb(esD, name + "wpl", [128, PLE // 128, 512], BF16)
                    sg = [sb(esD, f"{name}sg{i}", [128, 512], F32) for i in range(2)]
                xp_rr = [0]
                for st in range(TOK // T2):
                    tok0 = st * T2
                    load_actT(esD, lambda t: src_bf_d[tok0 + t * 128: tok0 + (t + 1) * 128, :], K_src, aT, src_bf_d, stage=stage, nt=NT2, q="sp")
                    if gate is not None:
                        load_actT(esD, lambda t: gate[0][tok0 + t * 128: tok0 + (t + 1) * 128, :], PLE, pT, None, stage=stage, nt=NT2, q="pool")
                    for c0 in range(0, D, 512):
                        xp = xpan[xp_rr[0] % 2]
                        xp_rr[0] += 1
                        S.dma("sp", xp[:, :, :], resid_d[tok0:tok0 + T2, c0:c0 + 512].rearrange("(t p) n -> p t n", p=128),
                              reads=[resid_d] if isinstance(resid_d, TB) else [], writes=[xp])
                        if gate is None:
                            def epi(t, c0_, cn, ps, xp=xp):
                                S.op("dve", lambda e: e.scalar_tensor_tensor(out=z[:, t, c0_:c0_ + 512], in0=xp[:, t, :], scalar=alpha, in1=ps[:, :],
                                                                             op0=ALU.mult, op1=ALU.add), [xp, ps], [z])
                        else:
                            S.dma("pool", wpl[:, :, :], wtile_kp(gate[1], 0, PLE // 128, c0, 512), writes=[wpl])

                            def epi(t, c0_, cn, ps, xp=xp):
                                s_ = sg[t % 2]
                                S.op("act", lambda e: e.activation(out=s_[:, :], in_=ps[:, :], func=AF.Sigmoid), [ps], [s_])
                                pp = PS[4 + t % 2]

                                def fn(e):
                                    for k in range(PLE // 128):
                                        inst = e.matmul(pp[:, :], pT[:, k, t * 128:(t + 1) * 128], wpl[:, k, :], start=(k == 0), stop=(k == PLE // 128 - 1))
                                    return inst
                                S.op("pe", fn, [pT, wpl], [pp])
                                S.op("dve", lambda e: e.tensor_tensor(out=s_[:, :], in0=s_[:, :], in1=pp[:, :], op=ALU.mult), [s_, pp], [s_])
                                S.op("dve", lambda e: e.scalar_tensor_tensor(out=z[:, t, c0_:c0_ + 512], in0=xp[:, t, :], scalar=alpha, in1=s_[:, :],
                                                                             op0=ALU.mult, op1=ALU.add), [xp, s_], [z])
                        gemm(aT, K_src // 128, w2d, [(c0, 512, "B", 0)], wring, epi_B=epi, tiles=range(NT2))
                    for t in range(NT2):
                        rows = slice(tok0 + t * 128, tok0 + (t + 1) * 128)
                        layer_norm(esD, z, z[:, t, :], g_bc, b_bc, zb, out32_tb[rows, :], out32_tb,
                                   outbf_tb[rows, :] if outbf_tb is not None else None, outbf_tb, zb)
                S.barrier()

        resid_ln_phase("D", mrg_d, D, w_out, x_d, 0, x1_d, x1b_d)

        NTT = TOK // 128
        NB = CAP // 128
        with ExitStack() as esE:
            gates = sb(esE, "gates", [128, NTT, NE], F32)
            maskb = sb(esE, "maskb", [128, NTT, NE], BF16)
            aux = sb(esE, "aux", [128, NTT, NE, 5], BF16)
            auxc = sb(esE, "auxc", [128, NTT, 3], BF16)
            slotf = sb(esE, "slotf", [128, NTT, NE], F32)
            utri = sb(esE, "utri", [128, 128], BF16)
            onesq = sb(esE, "onesq", [128, 128], BF16)
            iota = sb(esE, "iota", [128, CAP], F32)
            dummy = sb(esE, "dummy", [128, NB], F32)
            S.dma("pool", utri[:, :], c_utri[:, :], writes=[utri])
            S.op("dve", lambda e: e.memset(onesq[:, :], 1.0), writes=[onesq])
            S.dma("sp", iota[:, :], c_iota[:, :], writes=[iota])
            S.dma("sp", dummy[:, :], c_dummy[:, :], writes=[dummy])
            tkid = sb(esE, "tkid", [128, 2, NTT], F32)
            S.dma("sp", tkid[:, :, :], c_tokid.rearrange("p (a b) -> p a b", a=2), writes=[tkid])
            S.op("dve", lambda e: e.tensor_copy(out=auxc[:, :, 0], in_=tkid[:, 0, :]), [tkid], [auxc])
            S.op("dve", lambda e: e.tensor_copy(out=auxc[:, :, 1], in_=tkid[:, 1, :]), [tkid], [auxc])
            S.op("dve", lambda e: e.memset(auxc[:, :, 2], 1.0), writes=[auxc])
            for ex_ in range(NE):
                S.op("dve", lambda e, ex_=ex_: e.tensor_copy(out=aux[:, :, ex_, 0:3], in_=auxc[:, :, :]), [auxc], [aux])
            with ExitStack() as esE0:
                hT = sb(esE0, "hT", [128, KC, T], BF16)
                stage = [sb(esE0, "stageE0", [128, D], BF16)]
                wrt = sb(esE0, "wrt", [128, KC, NE], BF16)
                brt = load_bc(esE0, "brt", b_rt, NE)
                bdn = sb(esE0, "bdn", [NE, D], BF16)
                gT = sb(esE0, "gT", [NE, 128], BF16)
                gbf = sb(esE0, "gbf", [128, NE], BF16)
                lg = sb(esE0, "lg", [128, NE], F32)
                m8 = sb(esE0, "m8", [128, 16], F32)
                xin = [sb(esE0, f"xin{i}", [128, D], F32) for i in range(2)]
                S.op("dve", lambda e: e.memset(stage[0][:, :], 0.0), writes=[stage[0]])
                for b_ in range(NB):
                    S.dma("sp", x1b_d[TOK + b_ * 128: TOK + (b_ + 1) * 128, :], stage[0][:, :], reads=[stage[0]], writes=[x1b_d])
                S.dma("pool", wrt[:, :, :], w_rt.rearrange("(kc p) n -> p kc n", p=128), writes=[wrt])
                S.dma("pool", bdn[:, :], b_dn[:, :], writes=[bdn])
                for st in range(NST):
                    tok0 = st * T
                    load_actT(esE0, lambda t: x1b_d[tok0 + t * 128: tok0 + (t + 1) * 128, :], D, hT, x1b_d, stage=stage, q="sp")
                    for t in range(NT):
                        ti = st * NT + t
                        rows = slice(tok0 + t * 128, tok0 + (t + 1) * 128)
                        ps = PS[6 + t % 2]

                        def fn(e, t=t, ps=ps):
                            for k in range(KC):
                                inst = e.matmul(ps[:, 0:NE], hT[:, k, t * 128:(t + 1) * 128], wrt[:, k, :], start=(k == 0), stop=(k == KC - 1))
                            return inst
                        S.op("pe", fn, [hT, wrt], [ps])
                        gt = gates[:, ti, :]
                        S.op("dve", lambda e, ps=ps: e.tensor_tensor(out=lg[:, :], in0=ps[:, 0:NE], in1=brt[:, :], op=ALU.add), [ps, brt], [lg])
                        S.op("dve", lambda e: e.max(out=m8[:, 0:8], in_=lg[:, :]), [lg], [m8])
                        S.op("dve", lambda e: e.tensor_scalar(out=m8[:, 8:9], in0=m8[:, 0:1], scalar1=-1.0, scalar2=None, op0=ALU.mult), [m8], [m8])
                        S.op("dve", lambda e, gt=gt: e.tensor_scalar(out=gt, in0=lg[:, :], scalar1=m8[:, TOPK - 1:TOPK], scalar2=None, op0=ALU.is_ge), [lg, m8], [gates])
                        S.op("act", lambda e, ti=ti, gt=gt: e.copy(out=maskb[:, ti, :], in_=gt), [gates], [maskb])
                        S.op("act", lambda e: e.activation(out=lg[:, :], in_=lg[:, :], func=AF.Exp, bias=m8[:, 8:9], scale=1.0), [lg, m8], [lg])
                        S.op("dve", lambda e, gt=gt: e.tensor_tensor(out=gt, in0=gt, in1=lg[:, :], op=ALU.mult), [gates, lg], [gates])
                        S.op("dve", lambda e, gt=gt: e.reduce_sum(out=m8[:, 9:10], in_=gt, axis=AX.X), [gates], [m8])
                        S.op("dve", lambda e: e.reciprocal(out=m8[:, 10:11], in_=m8[:, 9:10]), [m8], [m8])
                        S.op("dve", lambda e, gt=gt: e.tensor_scalar(out=gt, in0=gt, scalar1=m8[:, 10:11], scalar2=None, op0=ALU.mult), [gates, m8], [gates])
                        S.op("act", lambda e, ti=ti, gt=gt: e.copy(out=aux[:, ti, :, 3], in_=gt), [gates], [aux])
                        S.op("dve", lambda e, ti=ti, gt=gt: e.tensor_tensor(out=aux[:, ti, :, 4], in0=gt, in1=aux[:, ti, :, 3], op=ALU.subtract), [gates, aux], [aux])
                        S.op("act", lambda e, gt=gt: e.copy(out=gbf[:, :], in_=gt), [gates], [gbf])
                        S.op("pe", lambda e, ps=ps: e.matmul(ps[0:NE, 128:256], gbf[:, :], ident[:, :], start=True, stop=True), [gbf, ident], [ps])
                        evac(gT[:, :], ps[0:NE, 128:256], [ps], [gT])
                        xi = xin[t % 2]
                        S.dma("sp", xi[:, :], x1_d[rows, :], reads=[x1_d], writes=[xi])
                        for c0 in range(0, D, 512):
                            pb = PS[4 + (c0 // 512) % 2]
                            S.op("pe", lambda e, pb=pb, c0=c0: e.matmul(pb[:, :], gT[:, :], bdn[:, c0:c0 + 512], start=True, stop=True), [gT, bdn], [pb])
                            S.op("dve", lambda e, pb=pb, c0=c0, xi=xi: e.scalar_tensor_tensor(out=xi[:, c0:c0 + 512], in0=xi[:, c0:c0 + 512], scalar=alpha, in1=pb[:, :],
                                                                                              op0=ALU.mult, op1=ALU.add), [xi, pb], [xi])
                        for c_, a_ in enumerate(acc_ds):
                            S.dma("sp", a_[rows, :], xi[:, c_ * RCH:(c_ + 1) * RCH], reads=[xi], writes=[a_])
                for ti in range(NTT):
                    ps = PS[ti % 2]

                    def fn(e, ti=ti, ps=ps):
                        inst = e.matmul(ps[:, 0:NE], utri[:, :], maskb[:, ti, :], start=True, stop=(ti == 0))
                        for tj in range(ti):
                            inst = e.matmul(ps[:, 0:NE], onesq[:, :], maskb[:, tj, :], start=False, stop=(tj == ti - 1))
                        return inst
                    S.op("pe", fn, [utri, onesq, maskb], [ps])
                    S.op("dve", lambda e, ti=ti, ps=ps: e.scalar_tensor_tensor(out=slotf[:, ti, :], in0=ps[:, 0:NE], scalar=1.0, in1=maskb[:, ti, :],
                                                                               op0=ALU.add, op1=ALU.mult), [ps, maskb], [slotf])
                    S.op("dve", lambda e, ti=ti: e.tensor_scalar(out=slotf[:, ti, :], in0=slotf[:, ti, :], scalar1=-1.0, scalar2=None, op0=ALU.add), [slotf], [slotf])
                S.barrier()
            with ExitStack() as esE1:
                aT = sb(esE1, "aTe", [128, KC, CAP], BF16)
                wgr = [sb(esE1, f"wg{i}", [128, KG, 2, PW], BF16) for i in range(3)]
                wdr = [sb(esE1, f"wd{i}", [128, FKC, 512], BF16) for i in range(2)]
                h1 = [sb(esE1, f"h1_{i}", [128, NB, PW], BF16) for i in range(2)]
                h1T = sb(esE1, "h1T", [128, FKC, CAP], BF16)
                bgu = [sb(esE1, "bgu0", [1, NPP, 2, PW], BF16)]
                sw = [sb(esE1, f"sw{i}", [128, 3, PW], F32) for i in range(2)]
                xg = [sb(esE1, f"xg{i}", [128, D], BF16) for i in range(2)]
                ysb = sb(esE1, "ysb", [128, NB, D], F32)
                Pm = [sb(esE1, f"Pm{i}", [128, CAP], BF16) for i in range(2)]
                ie = sb(esE1, "ie", [128, NB, 5], F32)
                idf = sb(esE1, "idf", [128, 2, NB], F32)
                idx = [sb(esE1, f"idx{i}", [128, NB], mybir.dt.int32) for i in range(2)]
                ge = [sb(esE1, f"ge{i}", [128, NB], F32) for i in range(2)]
                kg = KG
                ngrp = KC // kg
                wg_rr = 0
                wd_rr = 0
                h1_rr = 0
                pm_rr = 0
                xg_rr = 0
                for ex in range(NE):
                    ix = idx[ex % 2]
                    gx = ge[ex % 2]
                    for ti in range(NTT):
                        pm = Pm[pm_rr % 2]
                        pm_rr += 1
                        S.op("dve", lambda e, pm=pm, ti=ti, ex=ex: e.tensor_scalar(out=pm[:, :], in0=iota[:, :], scalar1=slotf[:, ti, ex:ex + 1], scalar2=None, op0=ALU.is_equal),
                             [iota, slotf], [pm])
                        for b_ in range(NB):
                            pb = PS[b_]
                            S.op("pe", lambda e, pm=pm, ti=ti, b_=b_, pb=pb, ex=ex: e.matmul(pb[:, 0:5], pm[:, b_ * 128:(b_ + 1) * 128], aux[:, ti, ex, :],
                                                                                             start=(ti == 0), stop=(ti == NTT - 1)), [pm, aux], [pb])
                            if ti == NTT - 1:
                                evac(ie[:, b_, :], pb[:, 0:5], [pb], [ie])
                    S.op("dve", lambda e: e.tensor_tensor(out=idf[:, 0, :], in0=ie[:, :, 2], in1=dummy[:, :], op=ALU.mult), [ie, dummy], [idf])
                    S.op("dve", lambda e: e.scalar_tensor_tensor(out=idf[:, 1, :], in0=ie[:, :, 0], scalar=128.0, in1=ie[:, :, 1], op0=ALU.mult, op1=ALU.add), [ie], [idf])
                    S.op("dve", lambda e: e.tensor_tensor(out=idf[:, 1, :], in0=idf[:, 1, :], in1=idf[:, 0, :], op=ALU.subtract), [idf], [idf])
                    S.op("dve", lambda e: e.tensor_tensor(out=idf[:, 1, :], in0=idf[:, 1, :], in1=dummy[:, :], op=ALU.add), [idf, dummy], [idf])
                    S.op("dve", lambda e, ix=ix: e.tensor_copy(out=ix[:, :], in_=idf[:, 1, :]), [idf], [ix])
                    S.op("dve", lambda e, gx=gx: e.tensor_tensor(out=gx[:, :], in0=ie[:, :, 3], in1=ie[:, :, 4], op=ALU.add), [ie], [gx])
                    for b_ in range(NB):
                        xg_ = xg[xg_rr % 2]
                        xg_rr += 1
                        S.idma(xg_[:, :], None, x1b_d[:, :], bass.IndirectOffsetOnAxis(ap=ix[:, b_:b_ + 1], axis=0),
                               reads=[x1b_d, ix], writes=[xg_])
                        for k0 in range(0, KC, 4):
                            kn = min(4, KC - k0)
                            pt = PS[6 + (k0 // 4) % 2]

                            def fnT(e, xg_=xg_, k0=k0, kn=kn, pt=pt):
                                for j in range(kn):
                                    inst = e.matmul(pt[:, j * 128:(j + 1) * 128], xg_[:, (k0 + j) * 128:(k0 + j + 1) * 128], ident[:, :], start=True, stop=True)
                                return inst
                            S.op("pe", fnT, [xg_, ident], [pt])
                            evac(aT[:, k0:k0 + kn, b_ * 128:(b_ + 1) * 128], pt[:, 0:kn * 128].rearrange("p (a b) -> p a b", b=128), [pt], [aT])
                    bg = bgu[0]
                    S.dma("pool", bg[0:1, :, 0, :], b_gu[ex:ex + 1, 0:FF].rearrange("o (a b) -> o a b", b=PW), writes=[bg])
                    S.dma("pool", bg[0:1, :, 1, :], b_gu[ex:ex + 1, FF:2 * FF].rearrange("o (a b) -> o a b", b=PW), writes=[bg])
                    wv = w_gu[ex].rearrange("(kc p) n -> p kc n", p=128)
                    for pp in range(NPP):
                        f0 = pp * PW
                        hp = h1[h1_rr % 2]
                        h1_rr += 1
                        for g in range(ngrp):
                            wt = wgr[wg_rr % 3]
                            wg_rr += 1
                            S.dma("pool", wt[:, 0:kg, 0, :], wv[:, g * kg:(g + 1) * kg, f0:f0 + PW], writes=[wt])
                            S.dma("pool", wt[:, 0:kg, 1, :], wv[:, g * kg:(g + 1) * kg, FF + f0:FF + f0 + PW], writes=[wt])
                            for t in range(NB):
                                ps = PS[t % 4] if NB <= 4 else PS[t % 6]
                                assert NB <= 6

                                def fn(e, g=g, wt=wt, t=t, ps=ps, pp=pp, bg=bg):
                                    if g == 0:
                                        e.matmul(ps[:, :], ones[0:1, :], bg[0:1, pp, :, :].rearrange("o a b -> o (a b)"), start=True, stop=False)
                                    for k in range(kg):
                                        inst = e.matmul(ps[:, :], aT[:, g * kg + k, t * 128:(t + 1) * 128], wt[:, k, :, :].rearrange("p a b -> p (a b)"),
                                                        start=False, stop=(g == ngrp - 1 and k == kg - 1))
                                    return inst
                                S.op("pe", fn, [aT, wt, bg, ones], [ps])
                                if g == ngrp - 1:
                                    s_ = sw[t % 2]
                                    S.op("dve", lambda e, ps=ps, s_=s_: e.tensor_scalar(out=s_[:, 0, :], in0=ps[:, 0:PW], scalar1=LIMIT, scalar2=None, op0=ALU.min), [ps], [s_])
                                    S.op("act", lambda e, s_=s_: e.activation(out=s_[:, 1, :], in_=s_[:, 0, :], func=AF.Sigmoid, scale=SW_ALPHA), [s_], [s_])
                                    S.op("dve", lambda e, ps=ps, s_=s_: e.tensor_scalar(out=s_[:, 2, :], in0=ps[:, PW:2 * PW], scalar1=-LIMIT, scalar2=LIMIT, op0=ALU.max, op1=ALU.min), [ps], [s_])
                                    S.op("dve", lambda e, s_=s_: e.scalar_tensor_tensor(out=s_[:, 2, :], in0=s_[:, 2, :], scalar=1.0, in1=s_[:, 0, :], op0=ALU.add, op1=ALU.mult), [s_], [s_])
                                    S.op("dve", lambda e, s_=s_, t=t, hp=hp, gx=gx: e.scalar_tensor_tensor(
                                        out=hp[:, t, :], in0=s_[:, 2, :], scalar=gx[:, t:t + 1], in1=s_[:, 1, :], op0=ALU.mult, op1=ALU.mult),
                                        [s_, gx], [hp])
                                    nkp = PW // 128
                                    pt = PS[6 + t % 2]

                                    def fnT2(e, hp=hp, t=t, pt=pt):
                                        for j in range(nkp):
                                            inst = e.matmul(pt[:, j * 128:(j + 1) * 128], hp[:, t, j * 128:(j + 1) * 128], ident[:, :], start=True, stop=True)
                                        return inst
                                    S.op("pe", fnT2, [hp, ident], [pt])
                                    evac(h1T[:, pp * nkp:(pp + 1) * nkp, t * 128:(t + 1) * 128], pt[:, 0:nkp * 128].rearrange("p (a b) -> p a b", b=128), [pt], [h1T])
                    wdv = w_dn[ex].rearrange("(kc p) n -> p kc n", p=128)
                    for c0 in range(0, D, 512):
                        wt = wdr[wd_rr % 2]
                        wd_rr += 1
                        S.dma("pool", wt[:, :, :], wdv[:, :, c0:c0 + 512], writes=[wt])
                        for t in range(NB):
                            ps = PS[4 + t % 2]

                            def fn(e, wt=wt, t=t, ps=ps):
                                for k in range(FKC):
                                    inst = e.matmul(ps[:, :], h1T[:, k, t * 128:(t + 1) * 128], wt[:, k, :], start=(k == 0), stop=(k == FKC - 1))
                                return inst
                            S.op("pe", fn, [h1T, wt], [ps])
                            evac(ysb[:, t, c0:c0 + 512], ps[:, :], [ps], [ysb])
                    for b_ in range(NB):
                        for c_, a_ in enumerate(acc_ds):
                            S.idma(a_[:, :], bass.IndirectOffsetOnAxis(ap=ix[:, b_:b_ + 1], axis=0), ysb[:, b_, c_ * RCH:(c_ + 1) * RCH], None,
                                   reads=[ysb, ix], writes=[a_], compute_op=ALU.add)
                S.barrier()
            with ExitStack() as esE2:
                g_bc = load_bc(esE2, "ln2g", ln_d[1][0], D)
                b_bc = load_bc(esE2, "ln2b", ln_d[1][1], D)
                zb = sb(esE2, "zb2", [128, D], BF16)
                zz = [sb(esE2, f"zz{i}", [128, D], F32) for i in range(2)]
                for ti in range(NTT):
                    rows = slice(ti * 128, (ti + 1) * 128)
                    z_ = zz[ti % 2]
                    for c_, a_ in enumerate(acc_ds):
                        S.dma("sp", z_[:, c_ * RCH:(c_ + 1) * RCH], a_[rows, :], reads=[a_], writes=[z_])
                    layer_norm(esE2, z_, z_[:, :], g_bc, b_bc, zb, x2_d[rows, :], x2_d, x2b_d[rows, :], x2b_d, zb)
                S.barrier()

        resid_ln_phase("F", x2b_d, D, w_pg, x2_d, 2, out_tb, None, gate=(p_d, w_ple))
        S.barrier()
    return nc


def host_consts(cfg, core):
    AH, RH = cfg["AH"], cfg["RH"]
    j = np.arange(128)[:, None]
    i = np.arange(128)[None, :]
    slopes = 2.0 ** (-8.0 * (np.arange(AH) + 1) / AH)
    tabA = np.zeros((128, 2, AH, 128), np.float64)
    for c in range(2):
        dist = (128 + i - j) if c == 0 else (i - j)
        valid = (dist >= 0) & (dist < 128)
        for h in range(AH):
            tabA[:, c, h, :] = np.where(valid, np.exp(-slopes[h] * dist), 0.0)
    nq = cfg["SEQ"] // cfg["TOK"]
    seq_start = (core % nq) == 0
    tabF = np.zeros((128, AH, 128)) if seq_start else tabA[:, 0].copy()
    gam = 1.0 - 2.0 ** (-5.0 - np.arange(RH))
    DT = np.zeros((128, RH, 128))
    qdec = np.zeros((128, RH, 128))
    kdec = np.zeros((128, RH))
    for h in range(RH):
        d = i - j
        DT[:, h, :] = np.where(d >= 0, gam[h] ** np.maximum(d, 0), 0.0) * DK ** -0.5
        qdec[:, h, :] = gam[h] ** (np.arange(128) + 1.0)[None, :]
        kdec[:, h] = gam[h] ** (127.0 - np.arange(128)) * DK ** -0.5
    f = lambda a: np.ascontiguousarray(a.reshape(128, -1).astype(np.float32))
    CAP, TOK = cfg["CAP"], cfg["TOK"]
    pidx = np.arange(128)
    extra = dict(
        c_utri=(pidx[:, None] < pidx[None, :]).astype(np.float32),
        c_iota=np.ascontiguousarray(np.broadcast_to(np.arange(CAP, dtype=np.float32)[None, :], (128, CAP))),
        c_dummy=(TOK + np.arange(CAP // 128)[None, :] * 128 + pidx[:, None]).astype(np.float32),
        c_tokid=np.concatenate([np.broadcast_to(np.arange(TOK // 128, dtype=np.float32)[None, :], (128, TOK // 128)),
                                np.broadcast_to(pidx[:, None].astype(np.float32), (128, TOK // 128))], axis=1).copy())
    return dict(**extra, c_ident=np.eye(128, dtype=np.float32), c_tabA=f(tabA), c_tabF=f(tabF), c_DT=f(DT), c_qdec=f(qdec), c_kdec=f(kdec))


def make_in_maps(cfg, inp):
    TOK, NPREV, SEQ, NC = cfg["TOK"], cfg["NPREV"], cfg["SEQ"], cfg["NCORE"]
    nq = SEQ // TOK
    x = np.asarray(inp["x"], np.float32)
    p = np.asarray(inp["p"], np.float32)[0]
    D = x.shape[-1]
    shared = {}
    for k in ("w_in", "w_att_out", "w_ret_out", "w_out", "w_router", "w_gate_up", "b_gate_up", "w_down", "b_down", "w_ple", "w_ple_gate"):
        shared[k] = np.ascontiguousarray(np.asarray(inp[k], np.float32)[0])
    for k in ("attn_sinks", "ret_norm_g", "ln1_g", "ln1_b", "ln2_g", "ln2_b", "ln3_g", "ln3_b", "b_router"):
        shared[k] = np.ascontiguousarray(np.asarray(inp[k], np.float32)[0][None, :])
    maps = []
    for c in range(NC):
        b, q = c // nq, c % nq
        m = dict(shared)
        m["x"] = np.ascontiguousarray(x[b, q * TOK:(q + 1) * TOK])
        xp = np.zeros((NPREV, D), np.float32)
        if q > 0:
            xp[NPREV - q * TOK:] = x[b, 0:q * TOK]
        m["xprev"] = xp
        m["p"] = np.ascontiguousarray(p[b, q * TOK:(q + 1) * TOK])
        m.update(host_consts(cfg, c))
        maps.append(m)
    return maps


def kernel(**inputs):
    cfg = FULL
    nc = build(cfg)
    maps = make_in_maps(cfg, inputs)
    res = run_bass_kernel_spmd(nc, maps, core_ids=list(range(cfg["NCORE"])))
    nq = cfg["SEQ"] // cfg["TOK"]
    out = np.zeros((cfg["BATCH"], cfg["SEQ"], cfg["D"]), np.float32)
    for c in range(cfg["NCORE"]):
        b, q = c // nq, c % nq
        out[b, q * cfg["TOK"]:(q + 1) * cfg["TOK"]] = res.results[c]["out"]
    return out
```

```python
import math
from contextlib import ExitStack
import numpy as np
import concourse.bass as bass
import concourse.mybir as mybir
from concourse.bass_utils import run_bass_kernel_spmd

F32 = mybir.dt.float32
BF16 = mybir.dt.bfloat16
ALU = mybir.AluOpType
AF = mybir.ActivationFunctionType
AX = mybir.AxisListType

FULL = dict(D=4096, AH=32, AKV=4, RH=8, NE=32, FF=1536, PLE=256, TOK=2048, NPREV=6144,
            SEQ=8192, BATCH=2, NCORE=8, CAP=384)
HD, DK, DV = 64, 128, 256
TOPK = 4
LN_EPS = 1e-5
LIMIT = 7.0
SW_ALPHA = 1.702
T = 512
NT = 4
PW = 256


class TB:
    def __init__(self, t, name):
        self.t = t
        self.name = name
        self.w = {}
        self.r = {}

    def __getitem__(self, k):
        return self.t[k]


class Sync:
    NDMA = 20

    def __init__(self, nc, es):
        self.nc = nc
        self.e = {}
        for nm, attr in (("pe", "tensor"), ("dve", "vector"), ("act", "scalar"), ("pool", "gpsimd"), ("sp", "sync")):
            sem = es.enter_context(nc.semaphore("s_" + nm))
            self.e[nm] = dict(eng=getattr(nc, attr), sem=sem, cnt=0, seen={})
        self.dq = {}
        for q in ("pool", "sp"):
            sems = [es.enter_context(nc.semaphore(f"d_{q}{i}")) for i in range(self.NDMA)]
            self.dq[q] = dict(sems=sems, val=[0] * self.NDMA, rr=0)

    def _wait(self, en, tok):
        sem, val, key = tok
        e = self.e[en]
        if e["seen"].get(key, 0) < val:
            e["eng"].wait_ge(sem, val)
            e["seen"][key] = val

    def _deps(self, en, reads, writes, skip_same):
        for b in reads:
            for key, tok in b.w.items():
                if skip_same and key == en:
                    continue
                self._wait(en, tok)
        for b in writes:
            for d in (b.w, b.r):
                for key, tok in d.items():
                    if skip_same and key == en:
                        continue
                    self._wait(en, tok)

    def _mark(self, tok, reads, writes):
        key = tok[2]
        for b in reads:
            b.r[key] = tok
        for b in writes:
            b.w[key] = tok

    def op(self, en, fn, reads=(), writes=()):
        e = self.e[en]
        self._deps(en, reads, writes, skip_same=(en == "pe"))
        inst = fn(e["eng"])
        e["cnt"] += 1
        inst.then_inc(e["sem"], 1)
        self._mark((e["sem"], e["cnt"], en), reads, writes)

    def dma(self, q, out, in_, reads=(), writes=(), **kw):
        self._deps(q, reads, writes, skip_same=False)
        d = self.dq[q]
        i = d["rr"]
        d["rr"] = (i + 1) % self.NDMA
        key = (q, i)
        if d["val"][i] > 0:
            self._wait(q, (d["sems"][i], d["val"][i], key))
        inst = self.e[q]["eng"].dma_start(out=out, in_=in_, **kw)
        d["val"][i] += 16
        inst.then_inc(d["sems"][i], 16)
        self._mark((d["sems"][i], d["val"][i], key), reads, writes)

    def idma(self, out, out_off, in_, in_off, reads=(), writes=(), **kw):
        q = "pool"
        self._deps(q, reads, writes, skip_same=False)
        d = self.dq[q]
        i = d["rr"]
        d["rr"] = (i + 1) % self.NDMA
        key = (q, i)
        if d["val"][i] > 0:
            self._wait(q, (d["sems"][i], d["val"][i], key))
        inst = self.e[q]["eng"].indirect_dma_start(out=out, out_offset=out_off, in_=in_, in_offset=in_off, **kw)
        d["val"][i] += 16
        inst.then_inc(d["sems"][i], 16)
        self._mark((d["sems"][i], d["val"][i], key), reads, writes)

    def barrier(self):
        toks = []
        for nm, e in self.e.items():
            if e["cnt"] > 0:
                toks.append((e["sem"], e["cnt"], nm))
        for q, d in self.dq.items():
            for i in range(self.NDMA):
                if d["val"][i] > 0:
                    toks.append((d["sems"][i], d["val"][i], (q, i)))
        for nm in self.e:
            for tok in toks:
                if tok[2] != nm:
                    self._wait(nm, tok)


def build(cfg):
    D, AH, AKV, RH, NE, FF, PLE = cfg["D"], cfg["AH"], cfg["AKV"], cfg["RH"], cfg["NE"], cfg["FF"], cfg["PLE"]
    TOK, NPREV = cfg["TOK"], cfg["NPREV"]
    G = AH // AKV
    KC = D // 128
    KG = min(16, KC)
    AQW, AKW, RQW, RVW = AH * HD, AKV * HD, RH * DK, RH * DV
    o_aq = 0
    o_ak = o_aq + AQW
    o_av = o_ak + AKW
    o_rq = o_av + AKW
    o_rk = o_rq + RQW
    o_rv = o_rk + RQW
    o_rg = o_rv + RVW
    o_ga = o_rg + RVW
    o_gr = o_ga + D
    INW = o_gr + D
    NST = TOK // T
    NPST = NPREV // T
    NPP = FF // PW
    FKC = FF // 128
    alpha = float((2 * 1) ** 0.25)
    gam = [1.0 - 2.0 ** (-5.0 - h) for h in range(RH)]
    g128 = [float(np.float32(g ** 128)) for g in gam]

    nc = bass.Bass("TRN2", target_bir_lowering=False)

    def din(name, shape):
        return nc.dram_tensor(name, list(shape), F32, kind="ExternalInput").ap()

    x_d = din("x", [TOK, D])
    xp_d = din("xprev", [NPREV, D])
    p_d = din("p", [TOK, PLE])
    w_in = din("w_in", [D, INW])
    sinks_d = din("attn_sinks", [1, AH])
    rng_d = din("ret_norm_g", [1, RVW])
    w_ao = din("w_att_out", [AQW, D])
    w_ro = din("w_ret_out", [RVW, D])
    w_out = din("w_out", [D, D])
    ln_d = [(din(f"ln{i}_g", [1, D]), din(f"ln{i}_b", [1, D])) for i in (1, 2, 3)]
    w_rt = din("w_router", [D, NE])
    b_rt = din("b_router", [1, NE])
    w_gu = din("w_gate_up", [NE, D, 2 * FF])
    b_gu = din("b_gate_up", [NE, 2 * FF])
    w_dn = din("w_down", [NE, FF, D])
    b_dn = din("b_down", [NE, D])
    w_ple = din("w_ple", [PLE, D])
    w_pg = din("w_ple_gate", [D, D])
    c_ident = din("c_ident", [128, 128])
    c_tabA = din("c_tabA", [128, 2 * AH * 128])
    c_tabF = din("c_tabF", [128, AH * 128])
    c_DT = din("c_DT", [128, RH * 128])
    c_qdec = din("c_qdec", [128, RH * 128])
    c_kdec = din("c_kdec", [128, RH])
    CAP = cfg["CAP"]
    c_utri = din("c_utri", [128, 128])
    c_iota = din("c_iota", [128, CAP])
    c_dummy = din("c_dummy", [128, CAP // 128])
    c_tokid = din("c_tokid", [128, 2 * (TOK // 128)])
    out_d = nc.dram_tensor("out", [TOK, D], F32, kind="ExternalOutput").ap()

    def scratch(name, shape, dt):
        return TB(nc.dram_tensor(name, list(shape), dt, kind="Internal").ap(), name)

    yatt_d = scratch("yatt_d", [TOK, AQW], BF16)
    yret_d = scratch("yret_d", [TOK, RVW], BF16)
    sga_d = scratch("sga_d", [TOK, D], F32)
    sgr_d = scratch("sgr_d", [TOK, D], F32)
    mrg_d = scratch("mrg_d", [TOK, D], BF16)
    srg_d = scratch("srg_d", [TOK, RVW], BF16)
    x1_d = scratch("x1_d", [TOK, D], F32)
    x1b_d = scratch("x1b_d", [TOK + cfg["CAP"], D], BF16)
    RCH = min(2048, D)
    acc_ds = [scratch(f"acc_d{c}", [TOK + cfg["CAP"], RCH], F32) for c in range(D // RCH)]
    x2_d = scratch("x2_d", [TOK, D], F32)
    x2b_d = scratch("x2b_d", [TOK, D], BF16)
    out_tb = TB(out_d, "out")

    with ExitStack() as es0:
        S = Sync(nc, es0)

        uniq = [0]

        def sb(es, name, shape, dt):
            uniq[0] += 1
            return TB(es.enter_context(nc.sbuf_tensor(f"{name}_{uniq[0]}", list(shape), dt)), name)

        PS = [TB(es0.enter_context(nc.psum_tensor(f"ps{i}", [128, 512], F32)), f"ps{i}") for i in range(8)]
        ident = sb(es0, "ident", [128, 128], BF16)
        ones = sb(es0, "ones", [1, 128], BF16)
        S.dma("pool", ident[:, :], c_ident[:, :], writes=[ident])
        S.op("dve", lambda e: e.memset(ones[:, :], 1.0), writes=[ones])
        evac_rr = [0]

        def evac(out_ap, in_ap, reads, writes):
            evac_rr[0] ^= 1
            if evac_rr[0]:
                S.op("act", lambda e: e.copy(out=out_ap, in_=in_ap), reads, writes)
            else:
                S.op("dve", lambda e: e.tensor_copy(out=out_ap, in_=in_ap), reads, writes)

        def load_actT(es_stage, src_ap_fn, K, actT, src_tb, tps=(6, 7), stage=None, nt=NT, q="pool"):
            kc_n = K // 128
            for t in range(nt):
                stg = stage[t % len(stage)]
                S.dma(q, stg[:, 0:K], src_ap_fn(t), reads=[src_tb] if src_tb else [], writes=[stg])
                for k0 in range(0, kc_n, 4):
                    kn = min(4, kc_n - k0)
                    ps = PS[tps[(k0 // 4) % 2]]

                    def fn(e, k0=k0, kn=kn, ps=ps, stg=stg):
                        for j in range(kn):
                            inst = e.matmul(ps[:, j * 128:(j + 1) * 128], stg[:, (k0 + j) * 128:(k0 + j + 1) * 128],
                                            ident[:, :], start=True, stop=True)
                        return inst
                    S.op("pe", fn, reads=[stg, ident], writes=[ps])
                    evac(actT[:, k0:k0 + kn, t * 128:(t + 1) * 128],
                         ps[:, 0:kn * 128].rearrange("p (a b) -> p a b", b=128), [ps], [actT])

        def wtile_kp(w_ap2d, k0, nk, c0, cn):
            return w_ap2d.rearrange("(kc p) n -> p kc n", p=128)[:, k0:k0 + nk, c0:c0 + cn]

        def gemm(actT, kc_n, w_ap2d, panels, wring, epi_B=None, epi_A=None, bias_fn=None, tiles=range(NT), psB=(0, 1, 2, 3), psA=(4, 5)):
            kg = min(KG, kc_n)
            ngrp = kc_n // kg
            for (c0, cn, kind, M) in panels:
                wts = []
                for g in range(ngrp):
                    wt = wring[wring_rr[0] % len(wring)]
                    wring_rr[0] += 1
                    S.dma("pool", wt[:, 0:kg, 0:cn], wtile_kp(w_ap2d, g * kg, kg, c0, cn), writes=[wt])
                    wts.append(wt)
                if kind == "B":
                    for g in range(ngrp):
                        wt = wts[g]
                        for t in tiles:
                            ps = PS[psB[t % len(psB)]]

                            def fn(e, g=g, wt=wt, t=t, ps=ps):
                                for k in range(kg):
                                    inst = e.matmul(ps[:, 0:cn], actT[:, g * kg + k, t * 128:(t + 1) * 128], wt[:, k, 0:cn],
                                                    start=(g == 0 and k == 0), stop=(g == ngrp - 1 and k == kg - 1))
                                return inst
                            S.op("pe", fn, reads=[actT, wt], writes=[ps])
                            if g == ngrp - 1:
                                epi_B(t, c0, cn, ps)
                else:
                    nblk = cn // M
                    for blk in range(nblk):
                        ps = PS[psA[blk % len(psA)]]

                        def fn(e, blk=blk, ps=ps):
                            for g in range(ngrp):
                                for k in range(kg):
                                    inst = e.matmul(ps[0:M, 0:T], wts[g][:, k, blk * M:(blk + 1) * M], actT[:, g * kg + k, 0:T],
                                                    start=(g == 0 and k == 0), stop=(g == ngrp - 1 and k == kg - 1))
                            return inst
                        S.op("pe", fn, reads=[actT] + wts, writes=[ps])
                        epi_A(c0 + blk * M, M, ps)

        wring_rr = [0]

        def layer_norm(es, z, zt_ap, g_bc, b_bc, junk, out32_ap, out32_tb, outbf_ap=None, outbf_tb=None, zb=None):
            st = ln_small
            S.op("dve", lambda e: e.reduce_sum(out=st[:, 0:1], in_=zt_ap, axis=AX.X), [z], [st])
            S.op("dve", lambda e: e.tensor_tensor(out=junk[:, 0:D], in0=zt_ap, in1=zt_ap, op=ALU.mult), [z], [junk])
            S.op("dve", lambda e: e.reduce_sum(out=st[:, 1:2], in_=junk[:, 0:D], axis=AX.X), [junk], [st])
            S.op("dve", lambda e: e.tensor_scalar(out=st[:, 2:4], in0=st[:, 0:2], scalar1=1.0 / D, scalar2=None, op0=ALU.mult), [st], [st])
            S.op("dve", lambda e: e.tensor_tensor(out=st[:, 4:5], in0=st[:, 2:3], in1=st[:, 2:3], op=ALU.mult), [st], [st])
            S.op("dve", lambda e: e.tensor_tensor(out=st[:, 5:6], in0=st[:, 3:4], in1=st[:, 4:5], op=ALU.subtract), [st], [st])
            S.op("dve", lambda e: e.tensor_scalar(out=st[:, 6:7], in0=st[:, 5:6], scalar1=LN_EPS, scalar2=None, op0=ALU.add), [st], [st])
            S.op("act", lambda e: e.sqrt(out=st[:, 6:7], in_=st[:, 6:7]), [st], [st])
            S.op("dve", lambda e: e.reciprocal(out=st[:, 6:7], in_=st[:, 6:7]), [st], [st])
            S.op("dve", lambda e: e.tensor_scalar(out=zt_ap, in0=zt_ap, scalar1=st[:, 2:3], scalar2=st[:, 6:7], op0=ALU.subtract, op1=ALU.mult), [z, st], [z])
            S.op("dve", lambda e: e.tensor_tensor(out=zt_ap, in0=zt_ap, in1=g_bc[:, :], op=ALU.mult), [z, g_bc], [z])
            S.op("dve", lambda e: e.tensor_tensor(out=zt_ap, in0=zt_ap, in1=b_bc[:, :], op=ALU.add), [z, b_bc], [z])
            S.dma("sp", out32_ap, zt_ap, reads=[z], writes=[out32_tb])
            if outbf_ap is not None:
                S.op("act", lambda e: e.copy(out=zb[:, 0:D], in_=zt_ap), [z], [zb])
                S.dma("sp", outbf_ap, zb[:, 0:D], reads=[zb], writes=[outbf_tb])

        ln_small = sb(es0, "ln_small", [128, 8], F32)

        def load_bc(es, name, src_ap, n):
            t_ = sb(es, name, [128, n], F32)
            S.dma("sp", t_[:, :], src_ap.partition_broadcast(128), writes=[t_])
            return t_

        with ExitStack() as esAB:
            xT = sb(esAB, "xT", [128, KC, T], BF16)
            stage = [sb(esAB, "stage0", [128, D], BF16)]
            wring = [sb(esAB, f"wr{i}", [128, KG, 512], BF16) for i in range(max(3, KC // KG))]
            kT = sb(esAB, "kT", [64, AKV, 128 + T], BF16)
            v1 = sb(esAB, "v1", [128, NT + 1, AKV, HD + 1], BF16)
            state = sb(esAB, "state", [128, RH, DV], F32)
            state_bf = sb(esAB, "state_bf", [128, RH, DV], BF16)
            kdec = sb(esAB, "kdec", [128, RH], F32)
            S.dma("sp", kdec[:, :], c_kdec[:, :], writes=[kdec])
            S.op("dve", lambda e: e.memset(v1[:, :, :, :], 1.0), writes=[v1])
            S.op("dve", lambda e: e.memset(state[:, :, :], 0.0), writes=[state])
            S.op("dve", lambda e: e.memset(state_bf[:, :, :], 0.0), writes=[state_bf])

            def kv_halo_epis():
                def epi_A(c, M, ps):
                    kvh = (c - o_ak) // HD
                    evac(kT[:, kvh, 0:128], ps[0:HD, T - 128:T], [ps], [kT])

                def epi_B(t, c0, cn, ps):
                    if t == NT - 1:
                        evac(v1[:, 0, :, 0:HD], ps[:, 0:AKW].rearrange("p (a b) -> p a b", b=HD), [ps], [v1])
                return epi_A, epi_B

            def ret_state_update(ksc, vsb, t, ps_banks=(6, 7)):
                for h0 in range(0, RH, 2):
                    ps = PS[ps_banks[(h0 // 2) % 2]]

                    def fn(e, h0=h0, ps=ps):
                        for j in range(2):
                            h = h0 + j
                            inst = e.matmul(ps[:, j * DV:(j + 1) * DV], ksc[:, t, h * DK:(h + 1) * DK], vsb[:, t, h * DV:(h + 1) * DV],
                                            start=True, stop=True)
                        return inst
                    S.op("pe", fn, reads=[ksc, vsb], writes=[ps])
                    for j in range(2):
                        h = h0 + j
                        S.op("dve", lambda e, h=h, j=j, ps=ps: e.scalar_tensor_tensor(
                            out=state[:, h, :], in0=state[:, h, :], scalar=g128[h], in1=ps[:, j * DV:(j + 1) * DV],
                            op0=ALU.mult, op1=ALU.add), [state, ps], [state])
                S.op("act", lambda e: e.copy(out=state_bf[:, :, :], in_=state[:, :, :]), [state], [state_bf])

            with ExitStack() as esA:
                ksc = sb(esA, "ksc", [128, NT, RQW], BF16)
                vsb = sb(esA, "vsb", [128, NT, RVW], BF16)
                for pst in range(NPST):
                    load_actT(esA, lambda t, pst=pst: xp_d[pst * T + t * 128: pst * T + (t + 1) * 128, :], D, xT, None, stage=stage)

                    def epi_B(t, c0, cn, ps):
                        if c0 < o_rv:
                            for hh in range(cn // DK):
                                h = (c0 - o_rk) // DK + hh
                                S.op("dve", lambda e, h=h, hh=hh: e.tensor_scalar(
                                    out=ksc[:, t, h * DK:(h + 1) * DK], in0=ps[:, hh * DK:(hh + 1) * DK], scalar1=kdec[:, h:h + 1],
                                    scalar2=None, op0=ALU.mult), [ps, kdec], [ksc])
                        else:
                            evac(vsb[:, t, c0 - o_rv:c0 - o_rv + cn], ps[:, 0:cn], [ps], [vsb])
                    panels = [(c, min(512, o_rg - c), "B", 0) for c in range(o_rk, o_rg, 512)]
                    panels = []
                    for c in range(o_rk, o_rv, 512):
                        panels.append((c, min(512, o_rv - c), "B", 0))
                    for c in range(o_rv, o_rg, 512):
                        panels.append((c, min(512, o_rg - c), "B", 0))
                    gemm(xT, KC, w_in, panels, wring, epi_B=epi_B)
                    if pst == NPST - 1:
                        eA, eB = kv_halo_epis()
                        gemm(xT, KC, w_in, [(o_ak, AKW, "A", HD)], wring, epi_A=eA)
                        gemm(xT, KC, w_in, [(o_av, AKW, "B", 0)], wring, epi_B=eB, tiles=[NT - 1])
                    for t in range(NT):
                        ret_state_update(ksc, vsb, t)
                S.barrier()

            for st in range(NST):
                tok0 = st * T
                load_actT(esAB, lambda t: x_d[tok0 + t * 128: tok0 + (t + 1) * 128, :], D, xT, None, stage=stage)
                with ExitStack() as esI:
                    qT = sb(esI, "qT", [64, AH, T], BF16)
                    tabA = sb(esI, "tabA", [128, 2, AH, 128], BF16)
                    tabF = sb(esI, "tabF", [128, AH, 128], BF16)
                    esink = sb(esI, "esink", [128, AH], F32)
                    e_sb = [sb(esI, f"e_sb{i}", [128, 512], BF16) for i in range(2)]
                    PT = [sb(esI, f"PT{i}", [128, 4, 128], BF16) for i in range(2)]
                    den = sb(esI, "den", [128, 8], F32)
                    yatt = [sb(esI, f"yatt{i}", [128, AQW], BF16) for i in range(2)]
                    S.dma("pool", tabA[:, :, :, :], c_tabA.rearrange("p (c h i) -> p c h i", c=2, h=AH), writes=[tabA])
                    S.dma("pool", tabF[:, :, :], c_tabF.rearrange("p (h i) -> p h i", h=AH), writes=[tabF])
                    S.dma("sp", esink[:, :], sinks_d.partition_broadcast(128), writes=[esink])
                    S.op("act", lambda e: e.activation(out=esink[:, :], in_=esink[:, :], func=AF.Exp), [esink], [esink])

                    def epi_Aq(c, M, ps):
                        if c < o_ak:
                            evac(qT[:, c // HD, :], ps[0:HD, 0:T], [ps], [qT])
                        else:
                            kvh = (c - o_ak) // HD
                            evac(kT[:, kvh, 128:128 + T], ps[0:HD, 0:T], [ps], [kT])

                    def epi_Bv(t, c0, cn, ps):
                        evac(v1[:, t + 1, :, 0:HD], ps[:, 0:AKW].rearrange("p (a b) -> p a b", b=HD), [ps], [v1])
                    panels = [(c, min(512, AQW - c), "A", HD) for c in range(0, AQW, 512)] + [(o_ak, AKW, "A", HD)]
                    gemm(xT, KC, w_in, panels, wring, epi_A=epi_Aq)
                    gemm(xT, KC, w_in, [(o_av, AKW, "B", 0)], wring, epi_B=epi_Bv)
                    for t in range(NT):
                        ya = yatt[t % 2]
                        for kvh in range(AKV):
                            for h0 in range(kvh * G, (kvh + 1) * G, 4):
                                nh = min(4, G)
                                for c in range(2):
                                    ps = PS[c]
                                    S.op("pe", lambda e, c=c, ps=ps, h0=h0, kvh=kvh, t=t: e.matmul(
                                        ps[:, 0:nh * 128], kT[:, kvh, (t + c) * 128:(t + c + 1) * 128],
                                        qT[:, h0:h0 + nh, t * 128:(t + 1) * 128], start=True, stop=True), [kT, qT], [ps])
                                    S.op("act", lambda e, c=c, ps=ps: e.activation(out=e_sb[c][:, 0:nh * 128], in_=ps[:, 0:nh * 128],
                                                                                   func=AF.Exp, scale=HD ** -0.5), [ps], [e_sb[c]])
                                    tab = tabF[:, h0:h0 + nh, :] if (st == 0 and t == 0 and c == 0) else tabA[:, c, h0:h0 + nh, :]
                                    S.op("dve", lambda e, c=c, tab=tab: e.tensor_tensor(
                                        out=PT[c][:, 0:nh, :], in0=e_sb[c][:, 0:nh * 128].rearrange("p (a b) -> p a b", b=128), in1=tab,
                                        op=ALU.mult), [e_sb[c], tabA, tabF], [PT[c]])
                                pso = PS[2 + ((h0 // 4) % 2)]

                                def fn(e, pso=pso, kvh=kvh, t=t):
                                    for hh in range(nh):
                                        e.matmul(pso[:, hh * 128:hh * 128 + HD + 1], PT[0][:, hh, :], v1[:, t, kvh, :], start=True, stop=False)
                                        inst = e.matmul(pso[:, hh * 128:hh * 128 + HD + 1], PT[1][:, hh, :], v1[:, t + 1, kvh, :], start=False, stop=True)
                                    return inst
                                S.op("pe", fn, reads=[PT[0], PT[1], v1], writes=[pso])
                                pv = pso[:, 0:nh * 128].rearrange("p (a b) -> p a b", b=128)
                                S.op("dve", lambda e, pv=pv, h0=h0: e.tensor_tensor(out=den[:, 0:nh], in0=pv[:, :, HD], in1=esink[:, h0:h0 + nh], op=ALU.add),
                                     [pso, esink], [den])
                                S.op("dve", lambda e: e.reciprocal(out=den[:, 4:4 + nh], in_=den[:, 0:nh]), [den], [den])
                                for hh in range(nh):
                                    h = h0 + hh
                                    S.op("act", lambda e, hh=hh, h=h, pso=pso, ya=ya: e.activation(
                                        out=ya[:, h * HD:(h + 1) * HD], in_=pso[:, hh * 128:hh * 128 + HD], func=AF.Copy,
                                        scale=den[:, 4 + hh:5 + hh]), [pso, den], [ya])
                        S.dma("sp", yatt_d[tok0 + t * 128: tok0 + (t + 1) * 128, :], ya[:, :], reads=[ya], writes=[yatt_d])
                    S.op("act", lambda e: e.copy(out=kT[:, :, 0:128], in_=kT[:, :, T:T + 128]), [kT], [kT])
                    S.op("dve", lambda e: e.tensor_copy(out=v1[:, 0, :, :], in_=v1[:, NT, :, :]), [v1], [v1])
                    S.barrier()
                with ExitStack() as esII:
                    rqT = sb(esII, "rqT", [128, RH, T], BF16)
                    rkT = sb(esII, "rkT", [128, RH, T], BF16)
                    ksc = sb(esII, "ksc2", [128, NT, RQW], BF16)
                    vsb = sb(esII, "vsb2", [128, NT, RVW], BF16)
                    srg = [sb(esII, f"srg{i}", [128, RVW], BF16) for i in range(2)]
                    gsb = [sb(esII, f"gsb{i}", [128, 512], BF16) for i in range(2)]
                    DT = sb(esII, "DT", [128, RH, 128], F32)
                    qdec = sb(esII, "qdec", [128, RH, 128], BF16)
                    normg = load_bc(esII, "normg", rng_d, RVW)
                    AT = sb(esII, "AT", [128, RH, 128], BF16)
                    rqs = sb(esII, "rqs", [128, RH, 128], BF16)
                    o_sb = sb(esII, "o_sb", [128, RH, DV], F32)
                    junk = stage[0]
                    rst = sb(esII, "rst", [128, 6, RH], F32)
                    yret = [sb(esII, "yret0", [128, RVW], BF16)]
                    gst = [sb(esII, f"gst{i}", [128, 512], F32) for i in range(2)]
                    S.dma("sp", DT[:, :, :], c_DT.rearrange("p (h i) -> p h i", h=RH), writes=[DT])
                    S.dma("pool", qdec[:, :, :], c_qdec.rearrange("p (h i) -> p h i", h=RH), writes=[qdec])

                    def epi_Ar(c, M, ps):
                        if c < o_rk:
                            evac(rqT[:, (c - o_rq) // DK, :], ps[:, 0:T], [ps], [rqT])
                        else:
                            evac(rkT[:, (c - o_rk) // DK, :], ps[:, 0:T], [ps], [rkT])
                    gst_rr = [0]

                    def epi_Br(t, c0, cn, ps):
                        if c0 < o_rv:
                            for hh in range(cn // DK):
                                h = (c0 - o_rk) // DK + hh
                                S.op("dve", lambda e, h=h, hh=hh: e.tensor_scalar(
                                    out=ksc[:, t, h * DK:(h + 1) * DK], in0=ps[:, hh * DK:(hh + 1) * DK], scalar1=kdec[:, h:h + 1],
                                    scalar2=None, op0=ALU.mult), [ps, kdec], [ksc])
                        elif c0 < o_rg:
                            evac(vsb[:, t, c0 - o_rv:c0 - o_rv + cn], ps[:, 0:cn], [ps], [vsb])
                        elif c0 < o_ga:
                            g_ = gsb[gst_rr[0] % 2]
                            gst_rr[0] += 1
                            S.op("act", lambda e: e.activation(out=g_[:, 0:cn], in_=ps[:, 0:cn], func=AF.Silu), [ps], [g_])
                            S.dma("sp", srg_d[tok0 + t * 128: tok0 + (t + 1) * 128, c0 - o_rg:c0 - o_rg + cn], g_[:, 0:cn], reads=[g_], writes=[srg_d])
                        else:
                            g_ = gst[gst_rr[0] % 2]
                            gst_rr[0] += 1
                            S.op("act", lambda e: e.activation(out=g_[:, 0:cn], in_=ps[:, 0:cn], func=AF.Sigmoid), [ps], [g_])
                            dst, cc = (sga_d, c0 - o_ga) if c0 < o_gr else (sgr_d, c0 - o_gr)
                            S.dma("sp", dst[tok0 + t * 128: tok0 + (t + 1) * 128, cc:cc + cn], g_[:, 0:cn], reads=[g_], writes=[dst])
                    panels = [(c, min(512, o_rv - c), "A", DK) for c in range(o_rq, o_rv, 512)]
                    gemm(xT, KC, w_in, panels, wring, epi_A=epi_Ar)
                    panels = []
                    for lo, hi in ((o_rk, o_rv), (o_rv, o_rg), (o_rg, o_ga), (o_ga, o_gr), (o_gr, INW)):
                        for c in range(lo, hi, 512):
                            panels.append((c, min(512, hi - c), "B", 0))
                    gemm(xT, KC, w_in, panels, wring, epi_B=epi_Br)
                    for t in range(NT):
                        tsl = slice(t * 128, (t + 1) * 128)
                        for hg in range(0, RH, 4):
                            nh = min(4, RH - hg)
                            ps = PS[(hg // 4) % 2]

                            def fn(e, hg=hg, nh=nh, ps=ps):
                                for j in range(nh):
                                    inst = e.matmul(ps[:, j * 128:(j + 1) * 128], rkT[:, hg + j, tsl], rqT[:, hg + j, tsl], start=True, stop=True)
                                return inst
                            S.op("pe", fn, [rkT, rqT], [ps])
                            S.op("dve", lambda e, hg=hg, nh=nh, ps=ps: e.tensor_tensor(
                                out=AT[:, hg:hg + nh, :], in0=ps[:, 0:nh * 128].rearrange("p (a b) -> p a b", b=128), in1=DT[:, hg:hg + nh, :],
                                op=ALU.mult), [ps, DT], [AT])
                        S.op("dve", lambda e: e.tensor_tensor(out=rqs[:, :, :], in0=rqT[:, :, tsl], in1=qdec[:, :, :], op=ALU.mult), [rqT, qdec], [rqs])
                        for h0 in range(0, RH, 2):
                            ps = PS[2 + (h0 // 2) % 4]

                            def fn(e, h0=h0, ps=ps):
                                for j in range(2):
                                    h = h0 + j
                                    e.matmul(ps[:, j * DV:(j + 1) * DV], AT[:, h, :], vsb[:, t, h * DV:(h + 1) * DV], start=True, stop=False)
                                    inst = e.matmul(ps[:, j * DV:(j + 1) * DV], rqs[:, h, :], state_bf[:, h, :], start=False, stop=True)
                                return inst
                            S.op("pe", fn, [AT, vsb, rqs, state_bf], [ps])
                            evac(o_sb[:, h0:h0 + 2, :], ps[:, 0:2 * DV].rearrange("p (a b) -> p a b", b=DV), [ps], [o_sb])
                        ret_state_update(ksc, vsb, t)
                        S.op("dve", lambda e: e.tensor_reduce(out=rst[:, 0, :], in_=o_sb[:, :, :], axis=AX.X, op=ALU.add), [o_sb], [rst])
                        jv = junk[:, 0:RVW].rearrange("p (a b) -> p a b", b=DV)
                        S.op("dve", lambda e: e.tensor_tensor(out=jv, in0=o_sb[:, :, :], in1=o_sb[:, :, :], op=ALU.mult), [o_sb], [junk])
                        S.op("dve", lambda e: e.tensor_reduce(out=rst[:, 1, :], in_=jv, axis=AX.X, op=ALU.add), [junk], [rst])
                        S.op("dve", lambda e: e.tensor_scalar(out=rst[:, 2:4, :], in0=rst[:, 0:2, :], scalar1=1.0 / DV, scalar2=None, op0=ALU.mult), [rst], [rst])
                        S.op("dve", lambda e: e.tensor_tensor(out=rst[:, 4, :], in0=rst[:, 2, :], in1=rst[:, 2, :], op=ALU.mult), [rst], [rst])
                        S.op("dve", lambda e: e.tensor_tensor(out=rst[:, 5, :], in0=rst[:, 3, :], in1=rst[:, 4, :], op=ALU.subtract), [rst], [rst])
                        S.op("dve", lambda e: e.tensor_scalar(out=rst[:, 4, :], in0=rst[:, 5, :], scalar1=LN_EPS, scalar2=None, op0=ALU.add), [rst], [rst])
                        S.op("act", lambda e: e.sqrt(out=rst[:, 4, :], in_=rst[:, 4, :]), [rst], [rst])
                        S.op("dve", lambda e: e.reciprocal(out=rst[:, 4, :], in_=rst[:, 4, :]), [rst], [rst])
                        for h in range(RH):
                            S.op("dve", lambda e, h=h: e.tensor_scalar(out=o_sb[:, h, :], in0=o_sb[:, h, :], scalar1=rst[:, 2, h:h + 1],
                                                                       scalar2=rst[:, 4, h:h + 1], op0=ALU.subtract, op1=ALU.mult), [o_sb, rst], [o_sb])
                        of = o_sb[:, :, :].rearrange("p a b -> p (a b)")
                        S.op("dve", lambda e: e.tensor_tensor(out=of, in0=of, in1=normg[:, :], op=ALU.mult), [o_sb, normg], [o_sb])
                        yr = yret[0]
                        sr = srg[t % 2]
                        S.dma("sp", sr[:, :], srg_d[tok0 + t * 128: tok0 + (t + 1) * 128, :], reads=[srg_d], writes=[sr])
                        S.op("dve", lambda e, yr=yr, sr=sr: e.tensor_tensor(out=yr[:, :], in0=of, in1=sr[:, :], op=ALU.mult), [o_sb, sr], [yr])
                        S.dma("sp", yret_d[tok0 + t * 128: tok0 + (t + 1) * 128, :], yr[:, :], reads=[yr], writes=[yret_d])
                    S.barrier()
            S.barrier()

        with ExitStack() as esC:
            yaT = sb(esC, "yaT", [128, AQW // 128, T], BF16)
            yrT = sb(esC, "yrT", [128, RVW // 128, T], BF16)
            stage = [sb(esC, f"stageC{i}", [128, max(AQW, RVW)], BF16) for i in range(2)]
            wring = [sb(esC, f"wrC{i}", [128, KG, 512], BF16) for i in range(3)]
            gpan = [sb(esC, f"gpan{i}", [128, NT, 512], F32) for i in range(2)]
            m_sb = sb(esC, "m_sb", [128, NT, 512], F32)
            mb = [sb(esC, f"mb{i}", [128, 512], BF16) for i in range(2)]
            for st in range(NST):
                tok0 = st * T
                load_actT(esC, lambda t: yatt_d[tok0 + t * 128: tok0 + (t + 1) * 128, :], AQW, yaT, yatt_d, stage=stage, q="sp")
                load_actT(esC, lambda t: yret_d[tok0 + t * 128: tok0 + (t + 1) * 128, :], RVW, yrT, yret_d, stage=stage, q="sp")
                for c0 in range(0, D, 512):
                    S.dma("sp", gpan[0][:, :, :], sga_d[tok0:tok0 + T, c0:c0 + 512].rearrange("(t p) n -> p t n", p=128), reads=[sga_d], writes=[gpan[0]])
                    S.dma("sp", gpan[1][:, :, :], sgr_d[tok0:tok0 + T, c0:c0 + 512].rearrange("(t p) n -> p t n", p=128), reads=[sgr_d], writes=[gpan[1]])

                    def epi1(t, c0_, cn, ps):
                        S.op("dve", lambda e: e.tensor_tensor(out=m_sb[:, t, :], in0=ps[:, :], in1=gpan[0][:, t, :], op=ALU.mult), [ps, gpan[0]], [m_sb])

                    def epi2(t, c0_, cn, ps):
                        S.op("dve", lambda e: e.tensor_tensor(out=gpan[1][:, t, :], in0=ps[:, :], in1=gpan[1][:, t, :], op=ALU.mult), [ps, gpan[1]], [gpan[1]])
                        m_ = mb[t % 2]
                        S.op("dve", lambda e: e.tensor_tensor(out=m_[:, :], in0=gpan[1][:, t, :], in1=m_sb[:, t, :], op=ALU.add), [gpan[1], m_sb], [m_])
                        S.dma("sp", mrg_d[tok0 + t * 128: tok0 + (t + 1) * 128, c0_:c0_ + 512], m_[:, :], reads=[m_], writes=[mrg_d])
                    gemm(yaT, AQW // 128, w_ao, [(c0, 512, "B", 0)], wring, epi_B=epi1, psB=(0, 1, 2, 3))
                    gemm(yrT, RVW // 128, w_ro, [(c0, 512, "B", 0)], wring, epi_B=epi2, psB=(4, 5, 0, 1))
            S.barrier()

        T2, NT2 = 512, 4

        def resid_ln_phase(name, src_bf_d, K_src, w2d, resid_d, ln_i, out32_tb, outbf_tb, gate=None):
            with ExitStack() as esD:
                aT = sb(esD, name + "aT", [128, K_src // 128, T2], BF16)
                stage = [sb(esD, name + "stg0", [128, K_src], BF16)]
                wring = [sb(esD, f"{name}wr{i}", [128, KG, 512], BF16) for i in range(2)]
                z = sb(esD, name + "z", [128, NT2, D], F32)
                xpan = [sb(esD, f"{name}xp{i}", [128, NT2, 512], F32) for i in range(2)]
                g_bc = load_bc(esD, name + "g", ln_d[ln_i][0], D)
                b_bc = load_bc(esD, name + "b", ln_d[ln_i][1], D)
                zb = sb(esD, name + "zb", [128, D], BF16)
                if gate is not None:
                    pT = sb(esD, name + "pT", [128, PLE // 128, T2], BF16)
                    wpl = sb(esD, name + "wpl", [128, PLE // 128, 512], BF16)
                    sg = [sb(esD, f"{name}sg{i}", [128, 512], F32) for i in range(2)]
                xp_rr = [0]
                for st in range(TOK // T2):
                    tok0 = st * T2
                    load_actT(esD, lambda t: src_bf_d[tok0 + t * 128: tok0 + (t + 1) * 128, :], K_src, aT, src_bf_d, stage=stage, nt=NT2, q="sp")
                    if gate is not None:
                        load_actT(esD, lambda t: gate[0][tok0 + t * 128: tok0 + (t + 1) * 128, :], PLE, pT, None, stage=stage, nt=NT2, q="pool")
                    for c0 in range(0, D, 512):
                        xp = xpan[xp_rr[0] % 2]
                        xp_rr[0] += 1
                        S.dma("sp", xp[:, :, :], resid_d[tok0:tok0 + T2, c0:c0 + 512].rearrange("(t p) n -> p t n", p=128),
                              reads=[resid_d] if isinstance(resid_d, TB) else [], writes=[xp])
                        if gate is None:
                            def epi(t, c0_, cn, ps, xp=xp):
                                S.op("dve", lambda e: e.scalar_tensor_tensor(out=z[:, t, c0_:c0_ + 512], in0=xp[:, t, :], scalar=alpha, in1=ps[:, :],
                                                                             op0=ALU.mult, op1=ALU.add), [xp, ps], [z])
                        else:
                            S.dma("pool", wpl[:, :, :], wtile_kp(gate[1], 0, PLE // 128, c0, 512), writes=[wpl])

                            def epi(t, c0_, cn, ps, xp=xp):
                                s_ = sg[t % 2]
                                S.op("act", lambda e: e.activation(out=s_[:, :], in_=ps[:, :], func=AF.Sigmoid), [ps], [s_])
                                pp = PS[4 + t % 2]

                                def fn(e):
                                    for k in range(PLE // 128):
                                        inst = e.matmul(pp[:, :], pT[:, k, t * 128:(t + 1) * 128], wpl[:, k, :], start=(k == 0), stop=(k == PLE // 128 - 1))
                                    return inst
                                S.op("pe", fn, [pT, wpl], [pp])
                                S.op("dve", lambda e: e.tensor_tensor(out=s_[:, :], in0=s_[:, :], in1=pp[:, :], op=ALU.mult), [s_, pp], [s_])
                                S.op("dve", lambda e: e.scalar_tensor_tensor(out=z[:, t, c0_:c0_ + 512], in0=xp[:, t, :], scalar=alpha, in1=s_[:, :],
                                                                             op0=ALU.mult, op1=ALU.add), [xp, s_], [z])
                        gemm(aT, K_src // 128, w2d, [(c0, 512, "B", 0)], wring, epi_B=epi, tiles=range(NT2))
                    for t in range(NT2):
                        rows = slice(tok0 + t * 128, tok0 + (t + 1) * 128)
                        layer_norm(esD, z, z[:, t, :], g_bc, b_bc, zb, out32_tb[rows, :], out32_tb,
                                   outbf_tb[rows, :] if outbf_tb is not None else None, outbf_tb, zb)
                S.barrier()

        resid_ln_phase("D", mrg_d, D, w_out, x_d, 0, x1_d, x1b_d)

        NTT = TOK // 128
        NB = CAP // 128
        with ExitStack() as esE:
            gates = sb(esE, "gates", [128, NTT, NE], F32)
            maskb = sb(esE, "maskb", [128, NTT, NE], BF16)
            aux = sb(esE, "aux", [128, NTT, NE, 5], BF16)
            auxc = sb(esE, "auxc", [128, NTT, 3], BF16)
            slotf = sb(esE, "slotf", [128, NTT, NE], F32)
            utri = sb(esE, "utri", [128, 128], BF16)
            onesq = sb(esE, "onesq", [128, 128], BF16)
            iota = sb(esE, "iota", [128, CAP], F32)
            dummy = sb(esE, "dummy", [128, NB], F32)
            S.dma("pool", utri[:, :], c_utri[:, :], writes=[utri])
            S.op("dve", lambda e: e.memset(onesq[:, :], 1.0), writes=[onesq])
            S.dma("sp", iota[:, :], c_iota[:, :], writes=[iota])
            S.dma("sp", dummy[:, :], c_dummy[:, :], writes=[dummy])
            tkid = sb(esE, "tkid", [128, 2, NTT], F32)
            S.dma("sp", tkid[:, :, :], c_tokid.rearrange("p (a b) -> p a b", a=2), writes=[tkid])
            S.op("dve", lambda e: e.tensor_copy(out=auxc[:, :, 0], in_=tkid[:, 0, :]), [tkid], [auxc])
            S.op("dve", lambda e: e.tensor_copy(out=auxc[:, :, 1], in_=tkid[:, 1, :]), [tkid], [auxc])
            S.op("dve", lambda e: e.memset(auxc[:, :, 2], 1.0), writes=[auxc])
            for ex_ in range(NE):
                S.op("dve", lambda e, ex_=ex_: e.tensor_copy(out=aux[:, :, ex_, 0:3], in_=auxc[:, :, :]), [auxc], [aux])
            with ExitStack() as esE0:
                hT = sb(esE0, "hT", [128, KC, T], BF16)
                stage = [sb(esE0, "stageE0", [128, D], BF16)]
                wrt = sb(esE0, "wrt", [128, KC, NE], BF16)
                brt = load_bc(esE0, "brt", b_rt, NE)
                bdn = sb(esE0, "bdn", [NE, D], BF16)
                gT = sb(esE0, "gT", [NE, 128], BF16)
                gbf = sb(esE0, "gbf", [128, NE], BF16)
                lg = sb(esE0, "lg", [128, NE], F32)
                m8 = sb(esE0, "m8", [128, 16], F32)
                xin = [sb(esE0, f"xin{i}", [128, D], F32) for i in range(2)]
                S.op("dve", lambda e: e.memset(stage[0][:, :], 0.0), writes=[stage[0]])
                for b_ in range(NB):
                    S.dma("sp", x1b_d[TOK + b_ * 128: TOK + (b_ + 1) * 128, :], stage[0][:, :], reads=[stage[0]], writes=[x1b_d])
                S.dma("pool", wrt[:, :, :], w_rt.rearrange("(kc p) n -> p kc n", p=128), writes=[wrt])
                S.dma("pool", bdn[:, :], b_dn[:, :], writes=[bdn])
                for st in range(NST):
                    tok0 = st * T
                    load_actT(esE0, lambda t: x1b_d[tok0 + t * 128: tok0 + (t + 1) * 128, :], D, hT, x1b_d, stage=stage, q="sp")
                    for t in range(NT):
                        ti = st * NT + t
                        rows = slice(tok0 + t * 128, tok0 + (t + 1) * 128)
                        ps = PS[6 + t % 2]

                        def fn(e, t=t, ps=ps):
                            for k in range(KC):
                                inst = e.matmul(ps[:, 0:NE], hT[:, k, t * 128:(t + 1) * 128], wrt[:, k, :], start=(k == 0), stop=(k == KC - 1))
                            return inst
                        S.op("pe", fn, [hT, wrt], [ps])
                        gt = gates[:, ti, :]
                        S.op("dve", lambda e, ps=ps: e.tensor_tensor(out=lg[:, :], in0=ps[:, 0:NE], in1=brt[:, :], op=ALU.add), [ps, brt], [lg])
                        S.op("dve", lambda e: e.max(out=m8[:, 0:8], in_=lg[:, :]), [lg], [m8])
                        S.op("dve", lambda e: e.tensor_scalar(out=m8[:, 8:9], in0=m8[:, 0:1], scalar1=-1.0, scalar2=None, op0=ALU.mult), [m8], [m8])
                        S.op("dve", lambda e, gt=gt: e.tensor_scalar(out=gt, in0=lg[:, :], scalar1=m8[:, TOPK - 1:TOPK], scalar2=None, op0=ALU.is_ge), [lg, m8], [gates])
                        S.op("act", lambda e, ti=ti, gt=gt: e.copy(out=maskb[:, ti, :], in_=gt), [gates], [maskb])
                        S.op("act", lambda e: e.activation(out=lg[:, :], in_=lg[:, :], func=AF.Exp, bias=m8[:, 8:9], scale=1.0), [lg, m8], [lg])
                        S.op("dve", lambda e, gt=gt: e.tensor_tensor(out=gt, in0=gt, in1=lg[:, :], op=ALU.mult), [gates, lg], [gates])
                        S.op("dve", lambda e, gt=gt: e.reduce_sum(out=m8[:, 9:10], in_=gt, axis=AX.X), [gates], [m8])
                        S.op("dve", lambda e: e.reciprocal(out=m8[:, 10:11], in_=m8[:, 9:10]), [m8], [m8])
                        S.op("dve", lambda e, gt=gt: e.tensor_scalar(out=gt, in0=gt, scalar1=m8[:, 10:11], scalar2=None, op0=ALU.mult), [gates, m8], [gates])
                        S.op("act", lambda e, ti=ti, gt=gt: e.copy(out=aux[:, ti, :, 3], in_=gt), [gates], [aux])
                        S.op("dve", lambda e, ti=ti, gt=gt: e.tensor_tensor(out=aux[:, ti, :, 4], in0=gt, in1=aux[:, ti, :, 3], op=ALU.subtract), [gates, aux], [aux])
                        S.op("act", lambda e, gt=gt: e.copy(out=gbf[:, :], in_=gt), [gates], [gbf])
                        S.op("pe", lambda e, ps=ps: e.matmul(ps[0:NE, 128:256], gbf[:, :], ident[:, :], start=True, stop=True), [gbf, ident], [ps])
                        evac(gT[:, :], ps[0:NE, 128:256], [ps], [gT])
                        xi = xin[t % 2]
                        S.dma("sp", xi[:, :], x1_d[rows, :], reads=[x1_d], writes=[xi])
                        for c0 in range(0, D, 512):
                            pb = PS[4 + (c0 // 512) % 2]
                            S.op("pe", lambda e, pb=pb, c0=c0: e.matmul(pb[:, :], gT[:, :], bdn[:, c0:c0 + 512], start=True, stop=True), [gT, bdn], [pb])
                            S.op("dve", lambda e, pb=pb, c0=c0, xi=xi: e.scalar_tensor_tensor(out=xi[:, c0:c0 + 512], in0=xi[:, c0:c0 + 512], scalar=alpha, in1=pb[:, :],
                                                                                              op0=ALU.mult, op1=ALU.add), [xi, pb], [xi])
                        for c_, a_ in enumerate(acc_ds):
                            S.dma("sp", a_[rows, :], xi[:, c_ * RCH:(c_ + 1) * RCH], reads=[xi], writes=[a_])
                for ti in range(NTT):
                    ps = PS[ti % 2]

                    def fn(e, ti=ti, ps=ps):
                        inst = e.matmul(ps[:, 0:NE], utri[:, :], maskb[:, ti, :], start=True, stop=(ti == 0))
                        for tj in range(ti):
                            inst = e.matmul(ps[:, 0:NE], onesq[:, :], maskb[:, tj, :], start=False, stop=(tj == ti - 1))
                        return inst
                    S.op("pe", fn, [utri, onesq, maskb], [ps])
                    S.op("dve", lambda e, ti=ti, ps=ps: e.scalar_tensor_tensor(out=slotf[:, ti, :], in0=ps[:, 0:NE], scalar=1.0, in1=maskb[:, ti, :],
                                                                               op0=ALU.add, op1=ALU.mult), [ps, maskb], [slotf])
                    S.op("dve", lambda e, ti=ti: e.tensor_scalar(out=slotf[:, ti, :], in0=slotf[:, ti, :], scalar1=-1.0, scalar2=None, op0=ALU.add), [slotf], [slotf])
                S.barrier()
            with ExitStack() as esE1:
                aT = sb(esE1, "aTe", [128, KC, CAP], BF16)
                wgr = [sb(esE1, f"wg{i}", [128, KG, 2, PW], BF16) for i in range(3)]
                wdr = [sb(esE1, f"wd{i}", [128, FKC, 512], BF16) for i in range(2)]
                h1 = [sb(esE1, f"h1_{i}", [128, NB, PW], BF16) for i in range(2)]
                h1T = sb(esE1, "h1T", [128, FKC, CAP], BF16)
                bgu = [sb(esE1, "bgu0", [1, NPP, 2, PW], BF16)]
                sw = [sb(esE1, f"sw{i}", [128, 3, PW], F32) for i in range(2)]
                xg = [sb(esE1, f"xg{i}", [128, D], BF16) for i in range(2)]
                ysb = sb(esE1, "ysb", [128, NB, D], F32)
                Pm = [sb(esE1, f"Pm{i}", [128, CAP], BF16) for i in range(2)]
                ie = sb(esE1, "ie", [128, NB, 5], F32)
                idf = sb(esE1, "idf", [128, 2, NB], F32)
                idx = [sb(esE1, f"idx{i}", [128, NB], mybir.dt.int32) for i in range(2)]
                ge = [sb(esE1, f"ge{i}", [128, NB], F32) for i in range(2)]
                kg = KG
                ngrp = KC // kg
                wg_rr = 0
                wd_rr = 0
                h1_rr = 0
                pm_rr = 0
                xg_rr = 0
                st_ = dict(wg=0, wd=0, h1=0, pm=0, xg=0)

                def stage_idx(ex):
                    ix, gx = idx[ex % 2], ge[ex % 2]
                    for ti in range(NTT):
                        pm = Pm[st_["pm"] % 2]
                        st_["pm"] += 1
                        S.op("dve", lambda e, pm=pm, ti=ti: e.tensor_scalar(out=pm[:, :], in0=iota[:, :], scalar1=slotf[:, ti, ex:ex + 1], scalar2=None, op0=ALU.is_equal),
                             [iota, slotf], [pm])
                        for b_ in range(NB):
                            pb = PS[b_]
                            S.op("pe", lambda e, pm=pm, ti=ti, b_=b_, pb=pb: e.matmul(pb[:, 0:5], pm[:, b_ * 128:(b_ + 1) * 128], aux[:, ti, ex, :],
                                                                                      start=(ti == 0), stop=(ti == NTT - 1)), [pm, aux], [pb])
                            if ti == NTT - 1:
                                evac(ie[:, b_, :], pb[:, 0:5], [pb], [ie])
                    S.op("dve", lambda e: e.tensor_tensor(out=idf[:, 0, :], in0=ie[:, :, 2], in1=dummy[:, :], op=ALU.mult), [ie, dummy], [idf])
                    S.op("dve", lambda e: e.scalar_tensor_tensor(out=idf[:, 1, :], in0=ie[:, :, 0], scalar=128.0, in1=ie[:, :, 1], op0=ALU.mult, op1=ALU.add), [ie], [idf])
                    S.op("dve", lambda e: e.tensor_tensor(out=idf[:, 1, :], in0=idf[:, 1, :], in1=idf[:, 0, :], op=ALU.subtract), [idf], [idf])
                    S.op("dve", lambda e: e.tensor_tensor(out=idf[:, 1, :], in0=idf[:, 1, :], in1=dummy[:, :], op=ALU.add), [idf, dummy], [idf])
                    S.op("dve", lambda e: e.tensor_copy(out=ix[:, :], in_=idf[:, 1, :]), [idf], [ix])
                    S.op("dve", lambda e: e.tensor_tensor(out=gx[:, :], in0=ie[:, :, 3], in1=ie[:, :, 4], op=ALU.add), [ie], [gx])

                xg_of = {}

                def stage_gather(ex, b_):
                    ix = idx[ex % 2]
                    xg_ = xg[st_["xg"] % len(xg)]
                    st_["xg"] += 1
                    xg_of[(ex, b_)] = xg_
                    S.idma(xg_[:, :], None, x1b_d[:, :], bass.IndirectOffsetOnAxis(ap=ix[:, b_:b_ + 1], axis=0), reads=[x1b_d, ix], writes=[xg_])

                def stage_transpose(ex, b_):
                    xg_ = xg_of.pop((ex, b_))
                    for k0 in range(0, KC, 4):
                        kn = min(4, KC - k0)
                        pt = PS[6 + (k0 // 4) % 2]

                        def fnT(e, xg_=xg_, k0=k0, kn=kn, pt=pt):
                            for j in range(kn):
                                inst = e.matmul(pt[:, j * 128:(j + 1) * 128], xg_[:, (k0 + j) * 128:(k0 + j + 1) * 128], ident[:, :], start=True, stop=True)
                            return inst
                        S.op("pe", fnT, [xg_, ident], [pt])
                        evac(aT[:, k0:k0 + kn, b_ * 128:(b_ + 1) * 128], pt[:, 0:kn * 128].rearrange("p (a b) -> p a b", b=128), [pt], [aT])

                def stage_gateup(ex):
                    gx = ge[ex % 2]
                    bg = bgu[0]
                    S.dma("pool", bg[0:1, :, 0, :], b_gu[ex:ex + 1, 0:FF].rearrange("o (a b) -> o a b", b=PW), writes=[bg])
                    S.dma("pool", bg[0:1, :, 1, :], b_gu[ex:ex + 1, FF:2 * FF].rearrange("o (a b) -> o a b", b=PW), writes=[bg])
                    wv = w_gu[ex].rearrange("(kc p) n -> p kc n", p=128)
                    for pp in range(NPP):
                        f0 = pp * PW
                        hp = h1[st_["h1"] % 2]
                        st_["h1"] += 1
                        for g in range(ngrp):
                            wt = wgr[st_["wg"] % len(wgr)]
                            st_["wg"] += 1
                            S.dma("pool", wt[:, 0:kg, 0, :], wv[:, g * kg:(g + 1) * kg, f0:f0 + PW], writes=[wt])
                            S.dma("pool", wt[:, 0:kg, 1, :], wv[:, g * kg:(g + 1) * kg, FF + f0:FF + f0 + PW], writes=[wt])
                            for t in range(NB):
                                ps = PS[t % 4] if NB <= 4 else PS[t % 6]

                                def fn(e, g=g, wt=wt, t=t, ps=ps, pp=pp, bg=bg):
                                    if g == 0:
                                        e.matmul(ps[:, :], ones[0:1, :], bg[0:1, pp, :, :].rearrange("o a b -> o (a b)"), start=True, stop=False)
                                    for k in range(kg):
                                        inst = e.matmul(ps[:, :], aT[:, g * kg + k, t * 128:(t + 1) * 128], wt[:, k, :, :].rearrange("p a b -> p (a b)"),
                                                        start=False, stop=(g == ngrp - 1 and k == kg - 1))
                                    return inst
                                S.op("pe", fn, [aT, wt, bg, ones], [ps])
                                if g == ngrp - 1:
                                    s_ = sw[t % 2]
                                    S.op("dve", lambda e, ps=ps, s_=s_: e.tensor_scalar(out=s_[:, 0, :], in0=ps[:, 0:PW], scalar1=LIMIT, scalar2=None, op0=ALU.min), [ps], [s_])
                                    S.op("act", lambda e, s_=s_: e.activation(out=s_[:, 1, :], in_=s_[:, 0, :], func=AF.Sigmoid, scale=SW_ALPHA), [s_], [s_])
                                    S.op("dve", lambda e, ps=ps, s_=s_: e.tensor_scalar(out=s_[:, 2, :], in0=ps[:, PW:2 * PW], scalar1=-LIMIT, scalar2=LIMIT, op0=ALU.max, op1=ALU.min), [ps], [s_])
                                    S.op("dve", lambda e, s_=s_: e.scalar_tensor_tensor(out=s_[:, 2, :], in0=s_[:, 2, :], scalar=1.0, in1=s_[:, 0, :], op0=ALU.add, op1=ALU.mult), [s_], [s_])
                                    S.op("dve", lambda e, s_=s_, t=t, hp=hp, gx=gx: e.scalar_tensor_tensor(
                                        out=hp[:, t, :], in0=s_[:, 2, :], scalar=gx[:, t:t + 1], in1=s_[:, 1, :], op0=ALU.mult, op1=ALU.mult),
                                        [s_, gx], [hp])
                                    nkp = PW // 128
                                    pt = PS[6 + t % 2]

                                    def fnT2(e, hp=hp, t=t, pt=pt):
                                        for j in range(nkp):
                                            inst = e.matmul(pt[:, j * 128:(j + 1) * 128], hp[:, t, j * 128:(j + 1) * 128], ident[:, :], start=True, stop=True)
                                        return inst
                                    S.op("pe", fnT2, [hp, ident], [pt])
                                    evac(h1T[:, pp * nkp:(pp + 1) * nkp, t * 128:(t + 1) * 128], pt[:, 0:nkp * 128].rearrange("p (a b) -> p a b", b=128), [pt], [h1T])

                wd_of = {}

                def stage_wd_load(ex, ci):
                    wdv = w_dn[ex].rearrange("(kc p) n -> p kc n", p=128)
                    wt = wdr[st_["wd"] % len(wdr)]
                    st_["wd"] += 1
                    wd_of[(ex, ci)] = wt
                    S.dma("pool", wt[:, :, :], wdv[:, :, ci * 512:(ci + 1) * 512], writes=[wt])

                def stage_down(ex, ci):
                    wt = wd_of.pop((ex, ci))
                    c0 = ci * 512
                    for t in range(NB):
                        ps = PS[4 + t % 2]

                        def fn(e, wt=wt, t=t, ps=ps):
                            for k in range(FKC):
                                inst = e.matmul(ps[:, :], h1T[:, k, t * 128:(t + 1) * 128], wt[:, k, :], start=(k == 0), stop=(k == FKC - 1))
                            return inst
                        S.op("pe", fn, [h1T, wt], [ps])
                        evac(ysb[:, t, c0:c0 + 512], ps[:, :], [ps], [ysb])

                def stage_scatter(ex):
                    ix = idx[ex % 2]
                    for b_ in range(NB):
                        for c_, a_ in enumerate(acc_ds):
                            S.idma(a_[:, :], bass.IndirectOffsetOnAxis(ap=ix[:, b_:b_ + 1], axis=0), ysb[:, b_, c_ * RCH:(c_ + 1) * RCH], None,
                                   reads=[ysb, ix], writes=[a_], compute_op=ALU.add)

                NCI = D // 512
                NXG = len(xg)
                stage_idx(0)
                for b_ in range(NB):
                    stage_gather(0, b_)
                    stage_transpose(0, b_)
                for ex in range(NE):
                    stage_gateup(ex)
                    nxt = ex + 1 if ex + 1 < NE else None
                    if nxt is not None:
                        stage_idx(nxt)
                    npre = min(len(wdr), NCI)
                    for ci in range(npre):
                        stage_wd_load(ex, ci)
                    ng_early = min(NXG, NB) if nxt is not None else 0
                    for b_ in range(ng_early):
                        stage_gather(nxt, b_)
                    for ci in range(NCI):
                        stage_down(ex, ci)
                        if ci + npre < NCI:
                            stage_wd_load(ex, ci + npre)
                    stage_scatter(ex)
                    if nxt is not None:
                        for b_ in range(NB):
                            if b_ >= ng_early:
                                stage_gather(nxt, b_)
                            stage_transpose(nxt, b_)
                S.barrier()
            with ExitStack() as esE2:
                g_bc = load_bc(esE2, "ln2g", ln_d[1][0], D)
                b_bc = load_bc(esE2, "ln2b", ln_d[1][1], D)
                zb = sb(esE2, "zb2", [128, D], BF16)
                zz = [sb(esE2, f"zz{i}", [128, D], F32) for i in range(2)]
                for ti in range(NTT):
                    rows = slice(ti * 128, (ti + 1) * 128)
                    z_ = zz[ti % 2]
                    for c_, a_ in enumerate(acc_ds):
                        S.dma("sp", z_[:, c_ * RCH:(c_ + 1) * RCH], a_[rows, :], reads=[a_], writes=[z_])
                    layer_norm(esE2, z_, z_[:, :], g_bc, b_bc, zb, x2_d[rows, :], x2_d, x2b_d[rows, :], x2b_d, zb)
                S.barrier()

        resid_ln_phase("F", x2b_d, D, w_pg, x2_d, 2, out_tb, None, gate=(p_d, w_ple))
        S.barrier()
    return nc


def host_consts(cfg, core):
    AH, RH = cfg["AH"], cfg["RH"]
    j = np.arange(128)[:, None]
    i = np.arange(128)[None, :]
    slopes = 2.0 ** (-8.0 * (np.arange(AH) + 1) / AH)
    tabA = np.zeros((128, 2, AH, 128), np.float64)
    for c in range(2):
        dist = (128 + i - j) if c == 0 else (i - j)
        valid = (dist >= 0) & (dist < 128)
        for h in range(AH):
            tabA[:, c, h, :] = np.where(valid, np.exp(-slopes[h] * dist), 0.0)
    nq = cfg["SEQ"] // cfg["TOK"]
    seq_start = (core % nq) == 0
    tabF = np.zeros((128, AH, 128)) if seq_start else tabA[:, 0].copy()
    gam = 1.0 - 2.0 ** (-5.0 - np.arange(RH))
    DT = np.zeros((128, RH, 128))
    qdec = np.zeros((128, RH, 128))
    kdec = np.zeros((128, RH))
    for h in range(RH):
        d = i - j
        DT[:, h, :] = np.where(d >= 0, gam[h] ** np.maximum(d, 0), 0.0) * DK ** -0.5
        qdec[:, h, :] = gam[h] ** (np.arange(128) + 1.0)[None, :]
        kdec[:, h] = gam[h] ** (127.0 - np.arange(128)) * DK ** -0.5
    f = lambda a: np.ascontiguousarray(a.reshape(128, -1).astype(np.float32))
    CAP, TOK = cfg["CAP"], cfg["TOK"]
    pidx = np.arange(128)
    extra = dict(
        c_utri=(pidx[:, None] < pidx[None, :]).astype(np.float32),
        c_iota=np.ascontiguousarray(np.broadcast_to(np.arange(CAP, dtype=np.float32)[None, :], (128, CAP))),
        c_dummy=(TOK + np.arange(CAP // 128)[None, :] * 128 + pidx[:, None]).astype(np.float32),
        c_tokid=np.concatenate([np.broadcast_to(np.arange(TOK // 128, dtype=np.float32)[None, :], (128, TOK // 128)),
                                np.broadcast_to(pidx[:, None].astype(np.float32), (128, TOK // 128))], axis=1).copy())
    return dict(**extra, c_ident=np.eye(128, dtype=np.float32), c_tabA=f(tabA), c_tabF=f(tabF), c_DT=f(DT), c_qdec=f(qdec), c_kdec=f(kdec))


def make_in_maps(cfg, inp):
    TOK, NPREV, SEQ, NC = cfg["TOK"], cfg["NPREV"], cfg["SEQ"], cfg["NCORE"]
    nq = SEQ // TOK
    x = np.asarray(inp["x"], np.float32)
    p = np.asarray(inp["p"], np.float32)[0]
    D = x.shape[-1]
    shared = {}
    for k in ("w_in", "w_att_out", "w_ret_out", "w_out", "w_router", "w_gate_up", "b_gate_up", "w_down", "b_down", "w_ple", "w_ple_gate"):
        shared[k] = np.ascontiguousarray(np.asarray(inp[k], np.float32)[0])
    for k in ("attn_sinks", "ret_norm_g", "ln1_g", "ln1_b", "ln2_g", "ln2_b", "ln3_g", "ln3_b", "b_router"):
        shared[k] = np.ascontiguousarray(np.asarray(inp[k], np.float32)[0][None, :])
    maps = []
    for c in range(NC):
        b, q = c // nq, c % nq
        m = dict(shared)
        m["x"] = np.ascontiguousarray(x[b, q * TOK:(q + 1) * TOK])
        xp = np.zeros((NPREV, D), np.float32)
        if q > 0:
            xp[NPREV - q * TOK:] = x[b, 0:q * TOK]
        m["xprev"] = xp
        m["p"] = np.ascontiguousarray(p[b, q * TOK:(q + 1) * TOK])
        m.update(host_consts(cfg, c))
        maps.append(m)
    return maps


def kernel(**inputs):
    cfg = FULL
    nc = build(cfg)
    maps = make_in_maps(cfg, inputs)
    res = run_bass_kernel_spmd(nc, maps, core_ids=list(range(cfg["NCORE"])))
    nq = cfg["SEQ"] // cfg["TOK"]
    out = np.zeros((cfg["BATCH"], cfg["SEQ"], cfg["D"]), np.float32)
    for c in range(cfg["NCORE"]):
        b, q = c // nq, c % nq
        out[b, q * cfg["TOK"]:(q + 1) * cfg["TOK"]] = res.results[c]["out"]
    return out
```
